# Optimizing a Trainium2 kernel written in Bass

```python
import math
import jax, jax.numpy as jnp
from jax import lax
import numpy as np

D_MODEL = 1024
BATCH = 4
SEQ = 4096
DEPTH = 1

D_MIX = D_MODEL
W_A = D_MIX // 2
W_B = D_MIX - W_A
CHUNK = 128
HEADS_A = 8
DH_A = W_A // HEADS_A
SPATIAL_INIT_STD = 0.02
S5_GROUP = 16
S5_GROUPS = W_B // S5_GROUP
S5_STATE = 64
DT_MIN = 1e-3
DT_MAX = 1e-1
N_EXPERT_GROUPS = 4
EXPERTS_PER_GROUP = 8
TOP_K = 2
D_EXPERT = 256
PLE_DIM = 256
EPS = 1e-6

kernel_name = "hymba_gmlp_s5_hmoe_block"


def rmsnorm(x, g):
    xf = x.astype(jnp.float32)
    y = xf * lax.rsqrt(jnp.mean(xf * xf, axis=-1, keepdims=True) + EPS)
    return (y * g.astype(jnp.float32)).astype(x.dtype)


def gmlp_mixer(u, v, sgu_gain, w_s, b_s):
    bsz, seq, _ = u.shape
    u = jax.nn.gelu(u)
    v = rmsnorm(jax.nn.gelu(v), sgu_gain)
    v = v.reshape(bsz, seq // CHUNK, CHUNK, HEADS_A, DH_A)
    mask = jnp.tril(jnp.ones((CHUNK, CHUNK), dtype=v.dtype))
    z = jnp.einsum('hts,bcshd->bcthd', w_s * mask, v) + b_s.T[None, None, :, :, None]
    return u * z.reshape(bsz, seq, W_A)


def s5_mixer(s, a_re, a_im, log_dt, b_re, b_im, c_re, c_im, d_skip, w_glu, b_glu):
    dtype = s.dtype
    f32 = jnp.float32
    bsz, seq, _ = s.shape
    sf = s.astype(f32)
    lam = lax.complex(a_re.astype(f32), a_im.astype(f32))
    dt = jnp.exp(log_dt.astype(f32))[:, None]
    a_bar = jnp.exp(lam * dt)
    b_mat = lax.complex(b_re.astype(f32), b_im.astype(f32))
    b_bar = ((a_bar - 1.0) / lam)[..., None] * b_mat
    ug = sf.reshape(bsz, seq, S5_GROUPS, S5_GROUP).astype(jnp.complex64)
    bu = jnp.einsum('gpc,bsgc->bsgp', b_bar, ug)
    a_seq = jnp.broadcast_to(a_bar, bu.shape)

    def combine(left, right):
        a_l, b_l = left
        a_r, b_r = right
        return a_r * a_l, a_r * b_l + b_r

    _, states = lax.associative_scan(combine, (a_seq, bu), axis=1)
    c_mat = lax.complex(c_re.astype(f32), c_im.astype(f32))
    y = jnp.einsum('gcp,bsgp->bsgc', c_mat, states).real.reshape(bsz, seq, W_B)
    y = jax.nn.gelu(y + d_skip.astype(f32) * sf)
    gl = y @ w_glu.astype(f32) + b_glu.astype(f32)
    out = gl[..., :W_B] * jax.nn.sigmoid(gl[..., W_B:])
    return out.astype(dtype)


def hier_moe(xn, w_coarse, b_coarse, w_fine, b_fine, w_gate_e, w_up_e, w_down_e):
    bsz, seq, d = xn.shape
    n = bsz * seq
    xt = xn.reshape(n, d)
    xf = xt.astype(jnp.float32)
    pc = jax.nn.softmax(xf @ w_coarse.astype(jnp.float32) + b_coarse.astype(jnp.float32), axis=-1)
    g_sel = jnp.argmax(pc, axis=-1)
    p_g = jnp.max(pc, axis=-1)
    lf_all = jnp.einsum('nd,gde->nge', xf, w_fine.astype(jnp.float32)) + b_fine.astype(jnp.float32)
    lf = jnp.take_along_axis(lf_all, g_sel[:, None, None], axis=1)[:, 0]
    pf = jax.nn.softmax(lf, axis=-1)
    top_v, top_i = lax.top_k(pf, TOP_K)
    top_w = top_v / jnp.sum(top_v, axis=-1, keepdims=True) * p_g[:, None]
    comb_e = jnp.sum(jax.nn.one_hot(top_i, EXPERTS_PER_GROUP, dtype=jnp.float32) * top_w[..., None], axis=1)
    comb = (jax.nn.one_hot(g_sel, N_EXPERT_GROUPS, dtype=jnp.float32)[:, :, None] * comb_e[:, None, :]).astype(xt.dtype)
    y = jnp.zeros((n, d), dtype=xt.dtype)
    for g in range(N_EXPERT_GROUPS):
        h = jax.nn.silu(jnp.einsum('nd,edf->nef', xt, w_gate_e[g])) * jnp.einsum('nd,edf->nef', xt, w_up_e[g])
        y = y + jnp.einsum('nef,efd->nd', h * comb[:, g, :, None], w_down_e[g])
    return y.reshape(bsz, seq, d)


def setup_inputs(seed: int = 0) -> dict:
    key = jax.random.key(seed)
    ks = iter(jax.random.split(key, 40))
    L, D = DEPTH, D_MODEL
    G, P, C = S5_GROUPS, S5_STATE, S5_GROUP
    NG, EPG, F = N_EXPERT_GROUPS, EXPERTS_PER_GROUP, D_EXPERT
    nrm = lambda shape, std: jax.random.normal(next(ks), shape, jnp.float32) * std
    gain = lambda shape: 1.0 + nrm(shape, 0.02)
    n_idx = jnp.arange(P, dtype=jnp.float32)
    return {
        "x": nrm((BATCH, SEQ, D), 1.0),
        "p": nrm((L, BATCH, SEQ, PLE_DIM), 1.0),
        "norm1": gain((L, D)),
        "w_in": nrm((L, D, 2 * W_A + W_B), D ** -0.5),
        "sgu_norm": gain((L, W_A)),
        "w_spatial": nrm((L, HEADS_A, CHUNK, CHUNK), SPATIAL_INIT_STD),
        "b_spatial": gain((L, HEADS_A, CHUNK)),
        "a_re": -0.5 + nrm((L, G, P), 0.01),
        "a_im": math.pi * n_idx[None, None, :] + nrm((L, G, P), 0.01),
        "log_dt": jax.random.uniform(next(ks), (L, G), jnp.float32, math.log(DT_MIN), math.log(DT_MAX)),
        "b_re": nrm((L, G, P, C), (2 * C) ** -0.5),
        "b_im": nrm((L, G, P, C), (2 * C) ** -0.5),
        "c_re": nrm((L, G, C, P), (2 * P) ** -0.5),
        "c_im": nrm((L, G, C, P), (2 * P) ** -0.5),
        "d_skip": nrm((L, W_B), 1.0),
        "w_glu": nrm((L, W_B, 2 * W_B), W_B ** -0.5),
        "b_glu": nrm((L, 2 * W_B), 0.02),
        "out_norm_a": gain((L, W_A)),
        "out_norm_b": gain((L, W_B)),
        "w_out": nrm((L, D_MIX, D), D_MIX ** -0.5),
        "norm2": gain((L, D)),
        "w_coarse": nrm((L, D, NG), D ** -0.5),
        "b_coarse": nrm((L, NG), 0.01),
        "w_fine": nrm((L, NG, D, EPG), D ** -0.5),
        "b_fine": nrm((L, NG, EPG), 0.01),
        "w_gate_e": nrm((L, NG, EPG, D, F), D ** -0.5),
        "w_up_e": nrm((L, NG, EPG, D, F), D ** -0.5),
        "w_down_e": nrm((L, NG, EPG, F, D), F ** -0.5),
        "norm3": gain((L, D)),
        "w_ple_gate": nrm((L, D, D), D ** -0.5),
        "w_ple_proj": nrm((L, PLE_DIM, D), PLE_DIM ** -0.5),
        "final_norm": gain((D,)),
    }


def reference(x, p, norm1, w_in, sgu_norm, w_spatial, b_spatial, a_re, a_im, log_dt,
              b_re, b_im, c_re, c_im, d_skip, w_glu, b_glu, out_norm_a, out_norm_b,
              w_out, norm2, w_coarse, b_coarse, w_fine, b_fine, w_gate_e, w_up_e,
              w_down_e, norm3, w_ple_gate, w_ple_proj, final_norm):
    h = x
    for i in range(DEPTH):
        hn = rmsnorm(h, norm1[i])
        proj = hn @ w_in[i]
        u = proj[..., :W_A]
        v = proj[..., W_A:2 * W_A]
        s = proj[..., 2 * W_A:]
        o_a = gmlp_mixer(u, v, sgu_norm[i], w_spatial[i], b_spatial[i])
        o_b = s5_mixer(s, a_re[i], a_im[i], log_dt[i], b_re[i], b_im[i], c_re[i], c_im[i],
                       d_skip[i], w_glu[i], b_glu[i])
        mix = jnp.concatenate([rmsnorm(o_a, out_norm_a[i]), rmsnorm(o_b, out_norm_b[i])], axis=-1)
        h = h + mix @ w_out[i]
        h = h + hier_moe(rmsnorm(h, norm2[i]), w_coarse[i], b_coarse[i], w_fine[i], b_fine[i],
                         w_gate_e[i], w_up_e[i], w_down_e[i])
        gate = jax.nn.sigmoid(rmsnorm(h, norm3[i]) @ w_ple_gate[i])
        h = h + gate * (p[i] @ w_ple_proj[i])
    return rmsnorm(h, final_norm)
```

```python
import math
import numpy as np
import ml_dtypes
from contextlib import ExitStack
import concourse.bass as bass
import concourse.mybir as mybir
from concourse.bass_utils import run_bass_kernel_spmd

F32 = mybir.dt.float32
BF16 = mybir.dt.bfloat16
AF = mybir.ActivationFunctionType
ALU = mybir.AluOpType

SAME_ENG_WAIT = True
N_DMA_SEMS = 24
DEBUG = {}
STOP_AFTER = None
SUB = None
NO_HALO = False
N_EXPERTS_RUN = 32
EPS = 1e-6
NTOK = 2048
GELU = AF.Gelu_apprx_tanh


def _is_psum_key(k):
    if isinstance(k, tuple):
        k = k[0]
    return isinstance(k, str) and k.startswith("ps")


class Op:
    __slots__ = ("eng", "fn", "deps", "signal", "seq", "is_dma", "slot", "target", "idx")


class V:
    def __init__(self, ap, keys):
        self.ap = ap
        self.keys = tuple(keys) if isinstance(keys, (list, tuple)) else (keys,)

    def __getitem__(self, idx):
        return V(self.ap[idx], self.keys)

    def rk(self, *keys):
        return V(self.ap, keys)

    def re(self, pattern, **kw):
        return V(self.ap.rearrange(pattern, **kw), self.keys)

    def bc(self, shape):
        return V(self.ap.broadcast_to(list(shape)), self.keys)

    def us(self, d):
        return V(self.ap.unsqueeze(d), self.keys)


class Prog:
    def __init__(self, nc):
        self.nc = nc
        self.ops = []
        self.es = ExitStack()
        self.st = {}
        self.dma_uses = [0] * N_DMA_SEMS
        self.n_dma = 0
        self.n_dma_sw = 0
        self.uid = 0
        self.last = {}
        self.dmas_since_barrier = []

    def tile(self, shape, dtype, key=None, es=None):
        self.uid += 1
        name = f"t{self.uid}"
        t = (es or self.es).enter_context(self.nc.sbuf_tensor(name, list(shape), dtype))
        return V(t[:], key or name)

    def ptile(self, shape, dtype, key=None, es=None):
        self.uid += 1
        name = f"p{self.uid}"
        t = (es or self.es).enter_context(self.nc.psum_tensor(name, list(shape), dtype))
        return V(t[:], key or name)

    def _s(self, k):
        s = self.st.get(k)
        if s is None:
            s = self.st[k] = {"w": [], "r": [], "war": []}
        return s

    def add(self, eng, fn, reads=(), writes=(), pwrites=(), is_dma=False, extra_deps=()):
        op = Op()
        op.eng = eng
        op.fn = fn
        op.signal = False
        op.seq = 0
        op.is_dma = is_dma
        op.idx = len(self.ops)
        deps = set(extra_deps)
        for k in reads:
            s = self._s(k)
            deps.update(s["w"])
            if _is_psum_key(k):
                for r_ in s["r"]:
                    if self.ops[r_].eng != eng:
                        deps.add(r_)
            s["r"].append(op.idx)
        for k in writes:
            s = self._s(k)
            deps.update(s["r"])
            deps.update(s["w"])
            deps.update(s["war"])
            s["w"] = [op.idx]
            s["r"] = []
            s["war"] = []
        for k in pwrites:
            s = self._s(k)
            if s["r"]:
                s["war"] = list(s["r"]) + list(s["w"])
                s["r"] = []
                s["w"] = []
            deps.update(s["war"])
            s["w"].append(op.idx)
        deps.discard(op.idx)
        if is_dma:
            half = N_DMA_SEMS // 2
            if eng == "pool":
                op.slot = half + self.n_dma_sw % half
                self.n_dma_sw += 1
            else:
                op.slot = self.n_dma % half
                self.n_dma += 1
            self.dma_uses[op.slot] += 1
            op.target = 16 * self.dma_uses[op.slot]
            self.dmas_since_barrier.append(op.idx)
        elif fn is not None:
            self.last[eng] = op.idx
        op.deps = deps
        self.ops.append(op)
        return op

    def barrier(self):
        deps = set(self.last.values()) | set(self.dmas_since_barrier)
        self.dmas_since_barrier = []
        for e in ("pe", "act", "dve", "pool", "sp"):
            self.add(e, None, extra_deps=deps)
        self.st = {}

    def emit(self, final_wait_keys=()):
        nc = self.nc
        self.add("sp", None, reads=final_wait_keys)
        ops = self.ops

        def skip_same(dop, op):
            return dop.eng == op.eng and (not op.is_dma) and (dop.eng == "pe" or not SAME_ENG_WAIT)

        for op in ops:
            for d in op.deps:
                dop = ops[d]
                if dop.is_dma or skip_same(dop, op):
                    continue
                dop.signal = True
        cnt = {}
        for op in ops:
            if op.signal and not op.is_dma:
                cnt[op.eng] = cnt.get(op.eng, 0) + 1
                op.seq = cnt[op.eng]
        engs = ["pe", "act", "dve", "pool", "sp"]
        sems = {e: self.es.enter_context(nc.semaphore(f"sem_{e}")) for e in engs}
        dsem = [self.es.enter_context(nc.semaphore(f"dsem{i}")) for i in range(N_DMA_SEMS)]
        streams = {e: [op for op in ops if op.eng == e] for e in engs}

        def run(eng_name, eng):
            waited = {}
            for op in streams[eng_name]:
                need = {}
                for d in op.deps:
                    dop = ops[d]
                    if dop.is_dma:
                        key = ("d", dop.slot)
                        val = dop.target
                    else:
                        if skip_same(dop, op):
                            continue
                        key = ("e", dop.eng)
                        val = dop.seq
                    if val > need.get(key, 0):
                        need[key] = val
                if op.is_dma:
                    key = ("d", op.slot)
                    val = op.target - 16
                    if val > need.get(key, 0):
                        need[key] = val
                for key, val in need.items():
                    if waited.get(key, 0) >= val:
                        continue
                    waited[key] = val
                    sem = dsem[key[1]] if key[0] == "d" else sems[key[1]]
                    eng.wait_ge(sem, val)
                if op.fn is None:
                    continue
                ins = op.fn(eng)
                if op.is_dma:
                    ins.then_inc(dsem[op.slot], 16)
                elif op.signal:
                    ins.then_inc(sems[op.eng], 1)

        with nc.Block() as block:
            @block.tensor
            def _(e):
                run("pe", e)

            @block.scalar
            def _(e):
                run("act", e)

            @block.vector
            def _(e):
                run("dve", e)

            @block.gpsimd
            def _(e):
                run("pool", e)

            @block.sync
            def _(e):
                run("sp", e)
        self.es.close()

    def _rw(self, out, ins, pw):
        reads = []
        for x in ins:
            if isinstance(x, V):
                reads.extend(x.keys)
        if pw:
            return dict(reads=reads, pwrites=list(out.keys))
        return dict(reads=reads, writes=list(out.keys))

    @staticmethod
    def _a(x):
        return x.ap if isinstance(x, V) else x

    def tt(self, out, a, b, op, eng="dve", pw=False):
        self.add(eng, lambda e: e.tensor_tensor(out=out.ap, in0=a.ap, in1=b.ap, op=op), **self._rw(out, (a, b), pw))

    def ts(self, out, a, s1, op0, s2=None, op1=None, eng="dve", pw=False):
        kw = {}
        if op1 is not None:
            kw["op1"] = op1
        self.add(eng, lambda e: e.tensor_scalar(out=out.ap, in0=a.ap, scalar1=self._a(s1), scalar2=self._a(s2), op0=op0, **kw),
                 **self._rw(out, (a, s1, s2), pw))

    def stt(self, out, a, s, b, op0, op1, pw=False):
        self.add("dve", lambda e: e.scalar_tensor_tensor(out=out.ap, in0=a.ap, scalar=self._a(s), in1=b.ap, op0=op0, op1=op1),
                 **self._rw(out, (a, s, b), pw))

    def actf(self, out, a, func, scale=None, bias=None, accum=None, pw=False):
        kw = {}
        if scale is not None:
            kw["scale"] = self._a(scale)
        if bias is not None:
            kw["bias"] = self._a(bias)
        rw = self._rw(out, (a, scale, bias), pw)
        if accum is not None:
            kw["accum_out"] = accum.ap
            rw.setdefault("writes", [])
            rw["writes"] = list(rw["writes"]) + list(accum.keys)
        self.add("act", lambda e: e.activation(out=out.ap, in_=a.ap, func=func, **kw), **rw)

    def cp(self, out, a, eng="dve", pw=False):
        if eng == "act":
            self.add("act", lambda e: e.copy(out=out.ap, in_=a.ap), **self._rw(out, (a,), pw))
        else:
            self.add(eng, lambda e: e.tensor_copy(out=out.ap, in_=a.ap), **self._rw(out, (a,), pw))

    def mm(self, out, lhsT, rhs, start, stop):
        self.add("pe", lambda e: e.matmul(out.ap, lhsT=lhsT.ap, rhs=rhs.ap, start=start, stop=stop),
                 reads=list(lhsT.keys) + list(rhs.keys), pwrites=list(out.keys))

    def tr(self, out, a, ident):
        self.add("pe", lambda e: e.transpose(out=out.ap, in_=a.ap, identity=ident.ap),
                 reads=list(a.keys) + list(ident.keys), pwrites=list(out.keys))

    def scan(self, out, d0, d1, init):
        self.add("dve", lambda e: e.tensor_tensor_scan(out=out.ap, data0=d0.ap, data1=d1.ap, initial=self._a(init),
                                                        op0=ALU.mult, op1=ALU.add),
                 **self._rw(out, (d0, d1, init), True))

    def recip(self, out, a):
        self.add("dve", lambda e: e.reciprocal(out=out.ap, in_=a.ap), **self._rw(out, (a,), False))

    def dma(self, out, a, q="sp", pw=False, **kw):
        oap = self._a(out)
        iap = self._a(a)
        reads = list(a.keys) if isinstance(a, V) else []
        wk = list(out.keys) if isinstance(out, V) else ["OUT"]
        if pw or not isinstance(out, V):
            self.add(q, lambda e: e.dma_start(out=oap, in_=iap, **kw), reads=reads, pwrites=wk, is_dma=True)
        else:
            self.add(q, lambda e: e.dma_start(out=oap, in_=iap, **kw), reads=reads, writes=wk, is_dma=True)


def pipeline(nt, stages):
    ns = len(stages)
    for step in range(nt + ns - 1):
        for s_ in reversed(range(ns)):
            t = step - s_
            if 0 <= t < nt:
                stages[s_](t)


def build_program(nc, dbg_specs):
    P = Prog(nc)
    I = {}

    def din(name, shape, dt=F32):
        I[name] = nc.dram_tensor(name, list(shape), dt, kind="ExternalInput").ap()

    din("x", [NTOK, 1024]); din("xprev", [NTOK, 1024]); din("p", [NTOK, 256])
    din("w_in", [1024, 1536]); din("w_glu", [512, 1024]); din("w_out", [1024, 1024])
    din("w_pg", [1024, 1024]); din("w_pp", [256, 1024])
    din("wg", [32, 1024, 256]); din("wu", [32, 1024, 256]); din("wd", [32, 256, 1024])
    din("wr", [1024, 36]); din("br", [1, 36])
    din("gvec", [6, 1024])
    din("bsp", [128, 512]); din("wsT", [8, 128, 128]); din("maskS", [128, 128]); din("gT", [128, 8])
    din("are", [128, 16]); din("aim", [128, 16]); din("ldt", [128, 16])
    din("bre", [128, 256]); din("bim", [128, 256]); din("cre", [128, 256]); din("cim", [128, 256])
    din("maskM", [128, 128]); din("hmask", [128, 2])
    din("bglu", [1, 1024]); din("ident", [128, 128])
    out_d = nc.dram_tensor("out", [NTOK, 1024], F32, kind="ExternalOutput").ap()
    D = {}
    for name, shape in dbg_specs.items():
        D[name] = nc.dram_tensor(name, list(shape), F32, kind="ExternalOutput").ap()

    def dump(name, v, shape2d=None):
        if name in D:
            P.dma(D[name], v)

    ident = P.tile([128, 128], F32, "ident")
    identb = P.tile([128, 128], BF16, "identb")
    P.dma(ident, I["ident"])
    P.cp(identb, ident)
    gbc = P.tile([128, 1024], F32, "gbc")
    mixTb = P.tile([128, 4, NTOK], BF16, "mixTb")

    def load_gain(row, n=1024, col0=0):
        P.dma(gbc[:, 0:n], I["gvec"][row:row + 1, col0:col0 + n].partition_broadcast(128).rearrange("p a n -> p (a n)"))

    nhalf = P.tile([128, 1], F32, "nhalf")
    P.add("dve", lambda e: e.memset(nhalf.ap, -0.5), writes=["nhalf"])

    def rstd_from_ss(ss, n, tmp, out):
        P.ts(tmp, ss, 1.0 / n, ALU.mult, s2=EPS, op1=ALU.add, eng="pool")
        P.tt(out, tmp, nhalf, ALU.pow, eng="pool")

    def norm_tm(src, dst_bf, scratch, ss, tmp, rstd, lo_bf=None, xf=None):
        P.actf(scratch, src, AF.Square, accum=ss)
        rstd_from_ss(ss, 1024, tmp, rstd)
        if lo_bf is None:
            P.stt(dst_bf, src, rstd, gbc, ALU.mult, ALU.mult)
        else:
            P.stt(xf, src, rstd, gbc, ALU.mult, ALU.mult)
            P.cp(dst_bf, xf, eng="act")
            P.tt(lo_bf, xf, dst_bf, ALU.subtract)

    es_p1 = ExitStack()
    es_a = ExitStack()
    sm = lambda shape, key=None, dt=F32: P.tile(shape, dt, key, es=es_a)

    NSEG = 64
    NCH = 128
    rho = P.tile([128, 16], F32, "rho", es=es_a)
    Rr = P.tile([128, 16, NSEG], F32, "Rr", es=es_a); Ri = P.tile([128, 16, NSEG], F32, "Ri", es=es_a)
    QTp = P.tile([128, 32, 2, 128], BF16, "QTp", es=es_a)
    MT = P.tile([128, 32, 128], BF16, "MT", es=es_a)
    PT = P.tile([128, 16, 2, 128], BF16, "PT", es=es_a)
    es_s = ExitStack()
    sm = lambda shape, key=None, dt=F32: P.tile(shape, dt, key, es=es_s)
    hmask = sm([128, 2], "hmask"); P.dma(hmask, I["hmask"])
    are = sm([128, 16]); aim = sm([128, 16]); ldt = sm([128, 16])
    P.dma(are, I["are"]); P.dma(aim, I["aim"]); P.dma(ldt, I["ldt"])
    bre = sm([128, 16, 16]); bim = sm([128, 16, 16]); cre = sm([128, 16, 16]); cim = sm([128, 16, 16])
    for t, n in ((bre, "bre"), (bim, "bim"), (cre, "cre"), (cim, "cim")):
        P.dma(t.re("p a b -> p (a b)"), I[n])
    maskM = sm([128, 128]); P.dma(maskM, I["maskM"])
    ctmp = [sm([128, 2048]) for _ in range(4)]

    def view(t, shape):
        n = 1
        for d in shape[1:]:
            n *= d
        v = t[:, 0:n]
        if len(shape) == 3:
            return v.re("p (a b) -> p a b", a=shape[1])
        if len(shape) == 4:
            return v.re("p (a b c) -> p a b c", a=shape[1], b=shape[2])
        return v

    def newt(shape=(128, 16)):
        return sm(list(shape))

    def cmul(orr, oi, ar, ai, br_, bi_, shape):
        t1, t2, t3, t4 = [view(t, shape) for t in ctmp]
        P.tt(t1, ar, br_, ALU.mult); P.tt(t2, ai, bi_, ALU.mult)
        P.tt(t3, ar, bi_, ALU.mult); P.tt(t4, ai, br_, ALU.mult)
        P.tt(orr, t1, t2, ALU.subtract, pw=True); P.tt(oi, t3, t4, ALU.add, pw=True)

    MAGIC = 12582912.0
    kf0 = newt(); P.ts(kf0, ldt, 1.0 / math.log(2.0), ALU.mult)
    kf1 = newt(); P.ts(kf1, kf0, MAGIC, ALU.add)
    kf = newt(); P.ts(kf, kf1, -MAGIC, ALU.add)
    LN2_HI = 0.693359375; LN2_LO = -2.12194440e-4
    r0 = newt(); P.stt(r0, kf, -LN2_HI, ldt, ALU.mult, ALU.add)
    r1 = newt(); P.stt(r1, kf, -LN2_LO, r0, ALU.mult, ALU.add)
    ecoef = [1.0 / math.factorial(k_) for k_ in range(0, 12)]
    acc = newt(); P.ts(acc, r1, ecoef[-1], ALU.mult)
    for c_ in reversed(ecoef[1:-1]):
        nxt = newt(); P.stt(nxt, acc, c_, r1, ALU.add, ALU.mult); acc = nxt
    dt_ = newt(); P.ts(dt_, acc, 1.0, ALU.add)
    mneg = newt(); P.ts(mneg, kf, -1.0, ALU.mult)
    for bit in (16, 8, 4, 2, 1):
        bsel = newt(); P.ts(bsel, mneg, float(1 - bit), ALU.add, s2=0.0, op1=ALU.max)
        bb = newt(); P.ts(bb, bsel, 1.0, ALU.min)
        m2_ = newt(); P.stt(m2_, bb, -float(bit), mneg, ALU.mult, ALU.add); mneg = m2_
        fac = newt(); P.ts(fac, bb, 2.0 ** (-bit) - 1.0, ALU.mult, s2=1.0, op1=ALU.add)
        nd = newt(); P.tt(nd, dt_, fac, ALU.mult); dt_ = nd
    zr = newt(); zi = newt()
    P.tt(zr, are, dt_, ALU.mult); P.tt(zi, aim, dt_, ALU.mult)
    e0 = newt(); P.actf(e0, zr, AF.Exp, scale=1.0 / 16)
    xs = newt(); P.ts(xs, zi, 1.0 / 16, ALU.mult)
    u2 = newt(); P.tt(u2, xs, xs, ALU.mult)

    def horner(coefs, u):
        acc = newt(); P.ts(acc, u, coefs[-1], ALU.mult)
        for c in reversed(coefs[1:-1]):
            nxt = newt(); P.stt(nxt, acc, c, u, ALU.add, ALU.mult); acc = nxt
        return acc

    sc = [1.0] + [(-1.0) ** k / math.factorial(2 * k + 1) for k in range(1, 7)]
    cc = [1.0] + [(-1.0) ** k / math.factorial(2 * k) for k in range(1, 8)]
    ps_ = horner(sc, u2)
    sn = newt(); P.stt(sn, ps_, 1.0, xs, ALU.add, ALU.mult)
    pc_ = horner(cc, u2)
    cs = newt(); P.ts(cs, pc_, 1.0, ALU.add)
    wr_ = newt(); wi_ = newt()
    P.tt(wr_, e0, cs, ALU.mult); P.tt(wi_, e0, sn, ALU.mult)
    pw_hist = []
    for _ in range(4):
        nr = newt(); ni = newt(); t1 = newt(); t2 = newt()
        P.tt(t1, wr_, wr_, ALU.mult); P.tt(t2, wi_, wi_, ALU.mult); P.tt(nr, t1, t2, ALU.subtract)
        P.tt(t1, wr_, wi_, ALU.mult); P.ts(ni, t1, 2.0, ALU.mult)
        wr_, wi_ = nr, ni
    a_r, a_i = wr_, wi_
    nrr = newt(); P.ts(nrr, a_r, -1.0, ALU.add)
    den = newt(); t1 = newt(); t2 = newt()
    P.tt(t1, are, are, ALU.mult); P.tt(t2, aim, aim, ALU.mult); P.tt(den, t1, t2, ALU.add)
    rden = newt(); P.recip(rden, den)
    kr = newt(); ki = newt(); t3 = newt(); t4 = newt()
    P.tt(t1, nrr, are, ALU.mult); P.tt(t2, a_i, aim, ALU.mult); P.tt(t3, t1, t2, ALU.add); P.tt(kr, t3, rden, ALU.mult)
    P.tt(t1, a_i, are, ALU.mult); P.tt(t2, nrr, aim, ALU.mult); P.tt(t4, t1, t2, ALU.subtract); P.tt(ki, t4, rden, ALU.mult)
    S3 = (128, 16, 16)
    Bbr = newt(S3); Bbi = newt(S3)
    cmul(Bbr, Bbi, kr.us(2).bc(S3), ki.us(2).bc(S3), bre, bim, S3)
    pwr = newt((128, 16, 8)); pwi = newt((128, 16, 8))
    P.cp(pwr[:, :, 0:1], a_r.us(2), pw=True); P.cp(pwi[:, :, 0:1], a_i.us(2), pw=True)
    n = 1
    while n < 8:
        sh = (128, 16, n)
        cmul(pwr[:, :, n:2 * n], pwi[:, :, n:2 * n], pwr[:, :, 0:n], pwi[:, :, 0:n],
             pwr[:, :, n - 1:n].bc(sh), pwi[:, :, n - 1:n].bc(sh), sh)
        n *= 2
    rvr = newt((128, 16, 8)); rvi = newt((128, 16, 8))
    P.add("dve", lambda e: e.memset(rvr.ap[:, :, 7:8], 1.0), pwrites=list(rvr.keys))
    P.add("dve", lambda e: e.memset(rvi.ap[:, :, 7:8], 0.0), pwrites=list(rvi.keys))
    for i in range(7):
        P.cp(rvr[:, :, i:i + 1], pwr[:, :, 6 - i:7 - i], pw=True)
        P.cp(rvi[:, :, i:i + 1], pwi[:, :, 6 - i:7 - i], pw=True)
    A8r = pwr[:, :, 7]; A8i = pwi[:, :, 7]
    m2 = newt(); P.tt(t1, A8r, A8r, ALU.mult); P.tt(t2, A8i, A8i, ALU.mult); P.tt(m2, t1, t2, ALU.add)
    rm = newt(); P.recip(rm, m2)
    ivr = newt(); ivi = newt()
    P.tt(ivr, A8r, rm, ALU.mult); P.tt(t1, A8i, rm, ALU.mult); P.ts(ivi, t1, -1.0, ALU.mult)
    P.actf(rho, m2, AF.Sqrt)
    rrho = newt(); P.recip(rrho, rho)
    P.tt(Rr[:, :, 0:1], A8r.us(2), rrho.us(2), ALU.mult, pw=True)
    P.tt(Ri[:, :, 0:1], A8i.us(2), rrho.us(2), ALU.mult, pw=True)
    n = 1
    while n < NSEG:
        sh = (128, 16, n)
        cmul(Rr[:, :, n:2 * n], Ri[:, :, n:2 * n], Rr[:, :, 0:n], Ri[:, :, 0:n],
             Rr[:, :, n - 1:n].bc(sh), Ri[:, :, n - 1:n].bc(sh), sh)
        n *= 2
    S4 = (128, 16, 8, 16)
    PNr = newt(S4); PNi = newt(S4)
    cmul(PNr, PNi, rvr.us(3).bc(S4), rvi.us(3).bc(S4), Bbr.us(2).bc(S4), Bbi.us(2).bc(S4), S4)
    QNr = newt(S4); QNi = newt(S4)
    cmul(QNr, QNi, pwr.us(3).bc(S4), pwi.us(3).bc(S4), cre.us(2).bc(S4), cim.us(2).bc(S4), S4)
    QMr = newt(S4); QMi = newt(S4)
    cmul(QMr, QMi, QNr, QNi, ivr.us(2).us(3).bc(S4), ivi.us(2).us(3).bc(S4), S4)
    QMp = newt((128, 32, 2, 128))
    QTv = QTp.re("p (gp h) r c -> p gp h r c", h=2)
    QMv = QMp.re("p (gp h) r c -> p gp h r c", h=2)
    for h in range(2):
        hm = hmask[:, h:h + 1]
        P.ts(QTv[:, :, h, 0, :], QNr.re("p g j c -> p g (j c)"), hm, ALU.mult, pw=True)
        P.ts(QTv[:, :, h, 1, :], QNi.re("p g j c -> p g (j c)"), hm, ALU.mult, s2=-1.0, op1=ALU.mult, pw=True)
        P.ts(QMv[:, :, h, 0, :], QMr.re("p g j c -> p g (j c)"), hm, ALU.mult, pw=True)
        P.ts(QMv[:, :, h, 1, :], QMi.re("p g j c -> p g (j c)"), hm, ALU.mult, s2=-1.0, op1=ALU.mult, pw=True)
    with ExitStack() as es_sp:
        ps_m = [P.ptile([128, 4, 128], F32, f"ps_m{i}", es=es_sp) for i in range(2)]
        ps_t = [P.ptile([128, 4, 128], F32, f"ps_t{i}", es=es_sp) for i in range(2)]
        for q4 in range(8):
            pm = ps_m[q4 % 2]
            for k_ in range(4):
                g = q4 * 4 + k_
                gp = g // 2
                P.mm(pm[:, k_, :], PNr[:, gp].re("p i c -> p (i c)"), QMp[:, g, 0, :], True, False)
                P.mm(pm[:, k_, :], PNi[:, gp].re("p i c -> p (i c)"), QMp[:, g, 1, :], False, True)
            P.tt(MT[:, q4 * 4:(q4 + 1) * 4, :], pm, maskM.us(1).bc((128, 4, 128)), ALU.mult, pw=True)
        for q2 in range(8):
            pt_ = ps_t[q2 % 2]
            for k_ in range(2):
                gp = q2 * 2 + k_
                for ri, pn in ((0, PNr), (1, PNi)):
                    P.tr(pt_[:, 2 * k_ + ri, :], pn[:, gp].re("p i c -> p (i c)"), ident)
            P.cp(PT[:, q2 * 2:(q2 + 1) * 2].re("p a b c -> p (a b) c"), pt_, eng="act", pw=True)
        P.barrier()
    def dump_bf(name, v2d, ncols):
        if name not in D:
            return
        for i, c0 in enumerate(range(0, ncols, 2048)):
            n_ = min(2048, ncols - c0)
            tmp = ctmp[i % 4][:, 0:n_]
            P.cp(tmp, v2d[:, c0:c0 + n_])
            P.dma(D[name][:, c0:c0 + n_], tmp)

    dump_bf("MT", MT.re("p g c -> p (g c)"), 32 * 128)
    dump_bf("PT", PT.re("p a b c -> p (a b c)"), 16 * 2 * 128)
    dump_bf("QT", QTp.re("p a b c -> p (a b c)"), 32 * 2 * 128)
    if "Rr" in D:
        P.dma(D["Rr"], Rr.re("p a b -> p (a b)")); P.dma(D["Ri"], Ri.re("p a b -> p (a b)")); P.dma(D["rho"], rho)
    P.barrier()
    es_s.close()
    if STOP_AFTER == "setup":
        es_a.close(); es_p1.close()
        P.emit(final_wait_keys=["OUT"])
        return nc

    Wins = P.tile([128, 8, 512], BF16, "Wins", es=es_a)
    P.dma(Wins, I["w_in"][:, 1024:1536].rearrange("(c p) f -> p c f", p=128), q="pool")
    Wglu = P.tile([128, 4, 1024], BF16, "Wglu", es=es_a)
    P.dma(Wglu, I["w_glu"].rearrange("(c p) f -> p c f", p=128), q="pool")
    bglu_bc = P.tile([128, 1024], F32, "bglu_bc", es=es_a)
    P.dma(bglu_bc, I["bglu"].partition_broadcast(128).rearrange("p a n -> p (a n)"))
    dsk_bc = P.tile([128, 512], F32, "dsk_bc", es=es_a)
    P.dma(dsk_bc, I["gvec"][5:6, 512:1024].partition_broadcast(128).rearrange("p a n -> p (a n)"))
    gob_bc = P.tile([128, 512], F32, "gob_bc", es=es_a)
    P.dma(gob_bc, I["gvec"][4:5, 512:1024].partition_broadcast(128).rearrange("p a n -> p (a n)"))
    load_gain(0)

    xT_blk = P.tile([128, 8, 1024], BF16, "xTblk", es=es_a)
    gT1 = P.tile([128, 8], F32, "gT1", es=es_a); P.dma(gT1, I["gT"])
    Zb2 = [P.tile([128, 32, 8, 16], BF16, f"Zb{i}", es=es_a) for i in range(2)]
    Ub2 = [P.tile([128, 32, 128], BF16, f"Ub{i}", es=es_a) for i in range(2)]
    T1 = P.tile([128, 8, NSEG], F32, "T1", es=es_a); T2 = P.tile([128, 8, NSEG], F32, "T2", es=es_a)
    TA = P.tile([128, 8, NSEG], F32, "TA", es=es_a); TB = P.tile([128, 8, NSEG], F32, "TB", es=es_a)
    Xr2 = [P.tile([128, 16, NCH + 1], BF16, f"Xr{i}", es=es_a) for i in range(2)]
    Xi2 = [P.tile([128, 16, NCH + 1], BF16, f"Xi{i}", es=es_a) for i in range(2)]
    xin_r = P.tile([128, 16], F32, "xin_r", es=es_a); xin_i = P.tile([128, 16], F32, "xin_i", es=es_a)
    P.add("dve", lambda e: e.memset(xin_r.ap, 0.0), writes=["xin_r"])
    P.add("dve", lambda e: e.memset(xin_i.ap, 0.0), writes=["xin_i"])
    ds2 = [P.tile([128, 8, 8, 16], BF16, f"ds_bf{i}", es=es_a) for i in range(2)]
    ypre2 = [P.tile([128, 8, 128], F32, f"ypre{i}", es=es_a) for i in range(2)]
    y_bf = P.tile([128, 8, 512], BF16, "y_bf", es=es_a)
    obn = y_bf
    xst = [P.tile([128, 1024], F32, f"xst{i}", es=es_a) for i in range(2)]
    xnb = [P.tile([128, 1024], BF16, f"xnb{i}", es=es_a) for i in range(4)]
    sq_scr = P.tile([128, 1024], BF16, "sq_scr", es=es_a)
    gas = [P.tile([128, 512], F32, f"ga{i}", es=es_a) for i in range(3)]
    gss = [P.tile([128, 512], F32, f"gs{i}", es=es_a) for i in range(3)]
    sst3 = [P.tile([128, 1], F32, f"ssg{i}", es=es_a) for i in range(3)]
    stmp3 = [P.tile([128, 1], F32, f"stg{i}", es=es_a) for i in range(3)]
    srs3 = [P.tile([128, 1], F32, f"srg{i}", es=es_a) for i in range(3)]
    sst = [P.tile([128, 1], F32, f"ss{i}", es=es_a) for i in range(2)]
    stmp = [P.tile([128, 1], F32, f"stmp{i}", es=es_a) for i in range(2)]
    srs = [P.tile([128, 1], F32, f"srs{i}", es=es_a) for i in range(2)]
    cr1 = P.tile([128, 8], F32, "cr1", es=es_a); cr2 = P.tile([128, 8], F32, "cr2", es=es_a)

    es_ps = ExitStack()
    psN = P.ptile([128, 4, 512], BF16, "psN", es=es_ps)
    psZ = [P.ptile([128, 512], F32, f"psZa{i}", es=es_ps) for i in range(2)]
    psA = P.ptile([128, 2, 512], F32, "psA", es=es_ps)
    psB = P.ptile([128, 2, 512], F32, "psB", es=es_ps)
    psAf = psA.re("p a b -> p (a b)"); psBf = psB.re("p a b -> p (a b)")

    def psN_q(q):
        return psN[:, q, :].rk(("psN", q // 2))

    def psN_bank(b_):
        return psN[:, 2 * b_:2 * b_ + 2, :].re("p a b -> p (a b)").rk(("psN", b_))

    def A_front(blk):
        main = blk >= 2
        src = I["x"] if main else I["xprev"]
        t0 = (blk % 2) * 1024
        zb = Zb2[blk % 2]; ub = Ub2[blk % 2]
        for half in range(2):
            for tl in range(4):
                tt_i = half * 4 + tl
                xs_ = xst[tt_i % 2]
                P.dma(xs_, src[t0 + tt_i * 128: t0 + (tt_i + 1) * 128, :])
                k = tt_i % 2
                P.actf(sq_scr, xs_, AF.Square, accum=sst[k])
                rstd_from_ss(sst[k], 1024, stmp[k], srs[k])
                P.actf(xnb[tl], xs_, AF.Copy, scale=srs[k])
            for rnd in range(2):
                for dq in range(4):
                    dc = rnd * 4 + dq
                    for tl in range(4):
                        P.tr(psN_q(dq)[:, tl * 128:(tl + 1) * 128], xnb[tl][:, dc * 128:(dc + 1) * 128], identb)
                for dq in range(4):
                    dc = rnd * 4 + dq
                    if blk < 2 and dq % 2 == 1:
                        P.ts(xT_blk[:, dc, half * 512:(half + 1) * 512], psN_q(dq), gT1[:, dc:dc + 1], ALU.mult, pw=True)
                    else:
                        P.actf(xT_blk[:, dc, half * 512:(half + 1) * 512], psN_q(dq), AF.Copy, scale=gT1[:, dc:dc + 1], pw=True)
        for i in range(8):
            pz = psZ[i % 2]
            for dc in range(8):
                lhs = xT_blk[:, dc, :].re("p (c e) -> p c e", e=8)[:, :, i]
                P.mm(pz, lhs, Wins[:, dc, :], dc == 0, dc == 7)
            P.cp(zb[:, :, i, :], pz.re("p (g c) -> p g c", g=32), eng=("dve" if (blk == 0 or (blk == 1 and i % 2)) else "act"), pw=True)
        for g8 in range(4):
            pu2 = psN_bank(g8 % 2)
            for gl in range(8):
                g = g8 * 8 + gl
                P.tr(pu2[:, gl * 128:(gl + 1) * 128], zb[:, g].re("p i c -> p (i c)"), identb)
            P.cp(ub[:, g8 * 8:(g8 + 1) * 8, :].re("p a b -> p (a b)"), pu2, eng=("dve" if (blk == 0 or (blk == 1 and g8 % 2)) else "act"), pw=True)

    def A_level1(blk, halves=(0, 1)):
        main = blk >= 2
        ub = Ub2[blk % 2]
        Xr = Xr2[blk % 2]; Xi = Xi2[blk % 2]
        for hf in halves:
            gsl = slice(hf * 8, (hf + 1) * 8)
            for gq in range(8):
                gp = hf * 8 + gq
                for h in range(2):
                    g = 2 * gp + h
                    for ri, pp_ in ((0, psAf), (1, psBf)):
                        P.mm(pp_[64 * h:64 * h + 64, gq * 128:(gq + 1) * 128], PT[:, gp, ri, 64 * h:64 * h + 64], ub[:, g, :], True, True)
            if main:
                P.cp(Xr[:, gsl, 0:1], xin_r[:, gsl].us(2), pw=True); P.cp(Xi[:, gsl, 0:1], xin_i[:, gsl].us(2), pw=True)
            for seg in range(NCH // NSEG):
                csl = slice(seg * NSEG, (seg + 1) * NSEG)
                VrP = psAf.re("p (a b) -> p a b", a=8)[:, :, csl]
                ViP = psBf.re("p (a b) -> p a b", a=8)[:, :, csl]
                rr = Rr[:, gsl, :]; ri_ = Ri[:, gsl, :]
                P.tt(T1, rr, VrP, ALU.mult); P.tt(T2, rr, ViP, ALU.mult)
                P.tt(TA, ri_, ViP, ALU.mult); P.tt(TB, ri_, VrP, ALU.mult)
                P.tt(T1, T1, TA, ALU.add)
                P.tt(T2, T2, TB, ALU.subtract)
                for gq in range(8):
                    gp = hf * 8 + gq
                    rb = rho[:, gp:gp + 1].bc((128, NSEG))
                    P.scan(T1[:, gq, :], rb, T1[:, gq, :], xin_r[:, gp:gp + 1])
                    P.scan(T2[:, gq, :], rb, T2[:, gq, :], xin_i[:, gp:gp + 1])
                L = NSEG - 1
                P.tt(cr1, rr[:, :, L], T1[:, :, L], ALU.mult); P.tt(cr2, ri_[:, :, L], T2[:, :, L], ALU.mult)
                P.tt(xin_r[:, gsl], cr1, cr2, ALU.subtract, pw=True)
                P.tt(cr1, rr[:, :, L], T2[:, :, L], ALU.mult); P.tt(cr2, ri_[:, :, L], T1[:, :, L], ALU.mult)
                P.tt(xin_i[:, gsl], cr1, cr2, ALU.add, pw=True)
                if main:
                    xs0 = 1 + seg * NSEG
                    P.tt(TA, rr, T1, ALU.mult); P.tt(TB, ri_, T2, ALU.mult, eng="pool")
                    P.tt(Xr[:, gsl, xs0:xs0 + NSEG], TA, TB, ALU.subtract, pw=True)
                    P.tt(TA, rr, T2, ALU.mult); P.tt(TB, ri_, T1, ALU.mult, eng="pool")
                    P.tt(Xi[:, gsl, xs0:xs0 + NSEG], TA, TB, ALU.add, pw=True)

    def A_out(blk, part):
        t0 = (blk % 2) * 1024
        zb = Zb2[blk % 2]; ub = Ub2[blk % 2]
        Xr = Xr2[blk % 2]; Xi = Xi2[blk % 2]
        yT = ub.re("p (ct j) c -> p ct j c", ct=4)
        if part == 0:
            for ct in range(4):
                def pY(hh, ct=ct):
                    return psZ[hh] if ct % 2 == 0 else (psAf if hh == 0 else psBf)[:, 0:512]
                ds_ = ds2[ct % 2]; yp_ = ypre2[ct % 2]
                for gl in range(8):
                    g = ct * 8 + gl
                    gp = g // 2
                    o = pY(gl // 4)[:, (gl % 4) * 128:(gl % 4 + 1) * 128]
                    P.mm(o, Xr[:, gp, 0:NCH], QTp[:, g, 0, :], True, False)
                    P.mm(o, Xi[:, gp, 0:NCH], QTp[:, g, 1, :], False, False)
                    P.mm(o, ub[:, g, :], MT[:, g, :], False, True)
                P.tt(ds_, zb[:, ct * 8:(ct + 1) * 8], dsk_bc[:, ct * 128:(ct + 1) * 128].re("p (g c) -> p g c", g=8).us(2).bc((128, 8, 8, 16)), ALU.mult, eng="pool")
                ypv = yp_.re("p j (g c) -> p g j c", g=8)
                for hh in range(2):
                    yv = pY(hh).re("p (g j c) -> p g j c", g=4, j=8)
                    P.tt(ypv[:, hh * 4:(hh + 1) * 4], yv, ds_[:, hh * 4:(hh + 1) * 4], ALU.add, pw=True)
                P.actf(y_bf[:, :, ct * 128:(ct + 1) * 128], yp_, GELU, pw=True)
            return
        for ct in range(4):
            pq = psN_bank(ct % 2)
            for j in range(8):
                P.tr(pq[:, j * 128:(j + 1) * 128], y_bf[:, j, ct * 128:(ct + 1) * 128], identb)
            P.cp(yT[:, ct].re("p j c -> p (j c)"), pq, eng="act", pw=True)
        def pglu(j, hh):
            return (psZ[hh] if j % 2 == 0 else (psA if hh == 0 else psB)[:, 0, :])

        def g_s0(j):
            for hh in range(2):
                for ct in range(4):
                    P.mm(pglu(j, hh), yT[:, ct, j, :], Wglu[:, ct, hh * 512:(hh + 1) * 512], ct == 0, ct == 3)

        def g_s1(j):
            P.tt(gas[j % 3], pglu(j, 0), bglu_bc[:, 0:512], ALU.add)
            P.tt(gss[j % 3], pglu(j, 1), bglu_bc[:, 512:1024], ALU.add)
            P.actf(gss[j % 3], gss[j % 3], AF.Sigmoid)

        def g_s2(j):
            P.tt(gas[j % 3], gas[j % 3], gss[j % 3], ALU.mult)
            P.actf(sq_scr[:, 0:512], gas[j % 3], AF.Square, accum=sst3[j % 3])
            rstd_from_ss(sst3[j % 3], 512, stmp3[j % 3], srs3[j % 3])

        def g_s3(j):
            P.stt(obn[:, j, :], gas[j % 3], srs3[j % 3], gob_bc, ALU.mult, ALU.mult, pw=True)

        pipeline(8, [g_s0, g_s1, g_s2, g_s3])
        for ct in range(4):
            pq = psN_bank(ct % 2)
            for j in range(8):
                P.tr(pq[:, j * 128:(j + 1) * 128], obn[:, j, ct * 128:(ct + 1) * 128], identb)
            dst = mixTb[:, ct, t0:t0 + 1024].re("p (c j) -> p j c", j=8)
            P.cp(dst, pq.re("p (j c) -> p j c", j=8), eng="act", pw=True)

    if not NO_HALO:
        A_front(0)
        A_front(1)
        A_level1(0)
    A_front(2)
    if not NO_HALO:
        A_level1(1)
    A_front(3)
    A_level1(2)
    A_level1(3, halves=(0,))
    A_out(2, 0)
    A_level1(3, halves=(1,))
    A_out(2, 1)
    A_out(3, 0)
    A_out(3, 1)
    P.barrier()
    es_ps.close()
    if "mixTb" in D:
        mf = xst[0]
        for ct in range(4):
            for hh in range(2):
                P.cp(mf, mixTb[:, ct, hh * 1024:(hh + 1) * 1024])
                P.dma(D["mixTb"][:, ct * NTOK + hh * 1024: ct * NTOK + (hh + 1) * 1024], mf)
        P.barrier()
    es_a.close()
    if STOP_AFTER == "A":
        es_p1.close()
        P.emit(final_wait_keys=["OUT"])
        return nc

    h_tm = P.tile([128, 16, 1024], F32, "h")
    es_b = ExitStack()
    Winuv = P.tile([128, 8, 1024], BF16, "Winuv", es=es_b)
    P.dma(Winuv, I["w_in"][:, 0:1024].rearrange("(c p) f -> p c f", p=128), q="pool")
    Wout = P.tile([128, 8, 1024], BF16, "Wout", es=es_b)
    P.dma(Wout, I["w_out"].rearrange("(c p) f -> p c f", p=128), q="pool")
    bsp = P.tile([128, 512], F32, "bsp", es=es_b); P.dma(bsp, I["bsp"])
    sgu_bc = P.tile([128, 512], F32, "sgu_bc", es=es_b)
    P.dma(sgu_bc, I["gvec"][5:6, 0:512].partition_broadcast(128).rearrange("p a n -> p (a n)"))
    goa_bc = P.tile([128, 512], F32, "goa_bc", es=es_b)
    P.dma(goa_bc, I["gvec"][4:5, 0:512].partition_broadcast(128).rearrange("p a n -> p (a n)"))
    wsTf = P.tile([128, 8, 128], F32, "wsTf", es=es_b)
    P.dma(wsTf, I["wsT"].rearrange("h s t -> s h t"))
    maskS = P.tile([128, 128], F32, "maskS", es=es_b); P.dma(maskS, I["maskS"])
    wsTb = P.tile([128, 8, 128], BF16, "wsTb", es=es_b)
    P.tt(wsTb, wsTf, maskS.us(1).bc((128, 8, 128)), ALU.mult)
    def ring(n, shape, dt, name):
        return [P.tile(shape, dt, f"{name}{i}", es=es_b) for i in range(n)]

    ug = ring(3, [128, 512], F32, "ug"); vg = ring(2, [128, 512], F32, "vg")
    vbf = ring(2, [128, 512], BF16, "vbf"); oa = ring(2, [128, 512], F32, "oa")
    oan = ring(2, [128, 512], BF16, "oan"); mTa = ring(2, [128, 4, 128], BF16, "mTa")
    xsb = ring(2, [128, 1024], F32, "xsb"); xsb2 = ring(2, [128, 1024], F32, "xsc")
    xnbt = ring(2, [128, 1024], BF16, "xnbt"); xnTt = ring(2, [128, 8, 128], BF16, "xnTt")
    sqb = P.tile([128, 1024], BF16, "sqb", es=es_b)
    NS = 3
    ssb = [ring(NS, [128, 1], F32, f"ssb{j}_") for j in range(3)]
    stb = [ring(NS, [128, 1], F32, f"stb{j}_") for j in range(3)]
    srb = [ring(NS, [128, 1], F32, f"srb{j}_") for j in range(3)]
    es_ps = ExitStack()
    psU = [P.ptile([128, 512], F32, f"psU{i}", es=es_ps) for i in range(2)]
    psV = P.ptile([128, 512], F32, "psV", es=es_ps)
    psZ = P.ptile([128, 512], F32, "psZ", es=es_ps)
    psX = P.ptile([128, 8, 128], BF16, "psX", es=es_ps)
    psD = P.ptile([128, 1024], F32, "psD", es=es_ps)
    psTt = P.ptile([128, 8, 128], BF16, "psTt", es=es_ps)

    def rstd3(j, t, n):
        rstd_from_ss(ssb[j][t % NS], n, stb[j][t % NS], srb[j][t % NS])

    def b_s0(t):
        P.dma(xsb[t % 2], I["x"][t * 128:(t + 1) * 128, :])
        P.actf(sqb, xsb[t % 2], AF.Square, accum=ssb[0][t % NS])
        rstd3(0, t, 1024)

    def b_s1(t):
        P.stt(xnbt[t % 2], xsb[t % 2], srb[0][t % NS], gbc, ALU.mult, ALU.mult)
        for dc in range(8):
            P.tr(psTt[:, dc, :], xnbt[t % 2][:, dc * 128:(dc + 1) * 128], identb)
        P.cp(xnTt[t % 2], psTt, eng="dve")

    def b_s2(t):
        for dc in range(8):
            P.mm(psU[t % 2], xnTt[t % 2][:, dc, :], Winuv[:, dc, 0:512], dc == 0, dc == 7)
        for dc in range(8):
            P.mm(psV, xnTt[t % 2][:, dc, :], Winuv[:, dc, 512:1024], dc == 0, dc == 7)

    def b_s3(t):
        P.actf(vg[t % 2], psV, GELU)
        P.actf(ug[t % 3], psU[t % 2], GELU)
        P.actf(sqb[:, 0:512], vg[t % 2], AF.Square, accum=ssb[1][t % NS])
        rstd3(1, t, 512)

    def b_s4(t):
        P.stt(vbf[t % 2], vg[t % 2], srb[1][t % NS], sgu_bc, ALU.mult, ALU.mult)
        for hh in range(8):
            P.mm(psZ[:, hh * 64:(hh + 1) * 64], wsTb[:, hh, :], vbf[t % 2][:, hh * 64:(hh + 1) * 64], True, True)

    def b_s5(t):
        P.tt(oa[t % 2], psZ, bsp, ALU.add)
        P.tt(oa[t % 2], oa[t % 2], ug[t % 3], ALU.mult)
        P.actf(sqb[:, 0:512], oa[t % 2], AF.Square, accum=ssb[2][t % NS])
        rstd3(2, t, 512)

    def b_s6(t):
        P.stt(oan[t % 2], oa[t % 2], srb[2][t % NS], goa_bc, ALU.mult, ALU.mult)
        for ct in range(4):
            P.tr(psX[:, ct, :], oan[t % 2][:, ct * 128:(ct + 1) * 128], identb)
        P.cp(mTa[t % 2], psX[:, 0:4, :], eng="act")
        P.dma(xsb2[t % 2], I["x"][t * 128:(t + 1) * 128, :])

    def b_s7(t):
        cols = slice(t * 128, (t + 1) * 128)
        for hh in range(2):
            o = psD[:, hh * 512:(hh + 1) * 512]
            for kk in range(8):
                lhs = mTa[t % 2][:, kk, :] if kk < 4 else mixTb[:, kk - 4, cols]
                P.mm(o, lhs, Wout[:, kk, hh * 512:(hh + 1) * 512], kk == 0, kk == 7)
        P.tt(h_tm[:, t, :].rk(("h", t)), xsb2[t % 2], psD, ALU.add)

    pipeline(16, [b_s0, b_s1, b_s2, b_s3, b_s4, b_s5, b_s6, b_s7])
    P.barrier()
    es_ps.close()
    es_b.close()
    es_p1.close()
    if "h1" in D:
        P.dma(D["h1"].rearrange("(t p) d -> p t d", p=128), h_tm)
        P.barrier()
    if STOP_AFTER == "B":
        P.emit(final_wait_keys=["OUT"])
        return nc

    xnT = P.tile([128, 8, NTOK], BF16, "xnT")
    Wpg = P.tile([128, 8, 1024], BF16, "Wpg")
    Wpp = P.tile([128, 2, 1024], BF16, "Wpp")
    es_m = ExitStack()
    load_gain(1)
    br_bc = P.tile([128, 36], F32, "br_bc", es=es_m)
    P.dma(br_bc, I["br"].partition_broadcast(128).rearrange("p a n -> p (a n)"))
    logit = P.tile([128, 16, 36], F32, "logit", es=es_m)
    comb = P.tile([128, 16, 4, 8], F32, "comb", es=es_m)
    NB = 3
    Wg = [P.tile([128, 8, 256], BF16, f"Wg{i}", es=es_m) for i in range(NB)]
    Wu = [P.tile([128, 8, 256], BF16, f"Wu{i}", es=es_m) for i in range(NB)]
    Wd = [P.tile([128, 2, 1024], BF16, f"Wd{i}", es=es_m) for i in range(NB)]

    def load_expert(e):
        b = e % NB
        P.dma(Wg[b], I["wg"][e].rearrange("(c p) f -> p c f", p=128), q="pool")
        P.dma(Wu[b], I["wu"][e].rearrange("(c p) f -> p c f", p=128), q="pool")
        P.dma(Wd[b], I["wd"][e].rearrange("(c p) f -> p c f", p=128), q="pool")

    for e in range(min(NB - 1, N_EXPERTS_RUN)):
        load_expert(e)
    es_n = ExitStack()
    wrf = P.tile([128, 8, 36], F32, "wrf", es=es_n)
    P.dma(wrf, I["wr"].rearrange("(c p) f -> p c f", p=128))
    wrh = P.tile([128, 8, 36], BF16, "wrh", es=es_n); wrl = P.tile([128, 8, 36], BF16, "wrl", es=es_n)
    P.cp(wrh, wrf); P.tt(wrl, wrf, wrh, ALU.subtract)

    def ringn(n, shape, dt, name):
        return [P.tile(shape, dt, f"{name}{i}", es=es_n) for i in range(n)]

    xfs = ringn(3, [128, 1024], F32, "xf")
    sq2 = P.tile([128, 1024], BF16, "sq2", es=es_n)
    xhi = ringn(2, [128, 1024], BF16, "xhi"); xlo = ringn(2, [128, 1024], BF16, "xlo")
    loTt = ringn(2, [128, 8, 128], BF16, "loTt")
    ss2 = ringn(3, [128, 1], F32, "ss2_"); st2 = ringn(3, [128, 1], F32, "st2_"); sr2 = ringn(3, [128, 1], F32, "sr2_")
    es_ps = ExitStack()
    psH = [P.ptile([128, 8, 128], BF16, f"psH{i}", es=es_ps) for i in range(2)]
    psLo = [P.ptile([128, 8, 128], BF16, f"psLo{i}", es=es_ps) for i in range(2)]
    psLG = [P.ptile([128, 512], F32, f"psLG{i}", es=es_ps) for i in range(2)]

    def hk2(t):
        return h_tm[:, t, :].rk(("h", t))

    def xk2(t):
        return xnT[:, :, t * 128:(t + 1) * 128].rk(("xnT2", t // 4))

    def n_s0(t):
        P.actf(sq2, hk2(t), AF.Square, accum=ss2[t % 3])
        rstd_from_ss(ss2[t % 3], 1024, st2[t % 3], sr2[t % 3])

    def n_s1(t):
        P.stt(xfs[t % 3], hk2(t), sr2[t % 3], gbc, ALU.mult, ALU.mult)
        P.cp(xhi[t % 2], xfs[t % 3], eng="act")
        P.tt(xlo[t % 2], xfs[t % 3], xhi[t % 2], ALU.subtract)

    def n_s2(t):
        for dc in range(8):
            P.tr(psH[t % 2][:, dc, :], xhi[t % 2][:, dc * 128:(dc + 1) * 128], identb)
        for dc in range(8):
            P.tr(psLo[t % 2][:, dc, :], xlo[t % 2][:, dc * 128:(dc + 1) * 128], identb)
        P.add("act", lambda e, t=t: e.copy(out=xk2(t).ap, in_=psH[t % 2].ap), reads=list(psH[t % 2].keys), pwrites=list(xk2(t).keys))
        P.cp(loTt[t % 2], psLo[t % 2], eng="dve")

    def n_s3(t):
        terms = []
        for dc in range(8):
            terms += [(xk2(t)[:, dc, :], wrh[:, dc, :]), (xk2(t)[:, dc, :], wrl[:, dc, :]), (loTt[t % 2][:, dc, :], wrh[:, dc, :])]
        for n_, (l_, r_) in enumerate(terms):
            P.mm(psLG[t % 2][:, 0:36], l_, r_, n_ == 0, n_ == len(terms) - 1)
        P.tt(logit[:, t, :], psLG[t % 2][:, 0:36], br_bc, ALU.add, pw=True)

    pipeline(16, [n_s0, n_s1, n_s2, n_s3])
    P.barrier()
    es_ps.close()
    es_n.close()
    rt = lambda shape, key=None: P.tile(shape, F32, key, es=es_m)
    AXX = mybir.AxisListType.X

    def reduce_x(out, in_, op):
        P.add("dve", lambda e: e.tensor_reduce(out=out.ap, in_=in_.ap, axis=AXX, op=op),
              reads=list(in_.keys), writes=list(out.keys))

    lc = logit[:, :, 0:4]
    mxc = rt([128, 16]); reduce_x(mxc, lc, ALU.max)
    ec = rt([128, 16, 4]); P.tt(ec, lc, mxc.us(2).bc((128, 16, 4)), ALU.subtract)
    ohg = rt([128, 16, 4]); P.ts(ohg, ec, 1e30, ALU.mult, s2=1.0, op1=ALU.add)
    P.ts(ohg, ohg, 0.0, ALU.max, s2=1.0, op1=ALU.min)
    P.actf(ec, ec, AF.Exp)
    sec = rt([128, 16]); reduce_x(sec, ec, ALU.add)
    pg = rt([128, 16]); P.recip(pg, sec)
    lfa = logit[:, :, 4:36].re("p t (g e) -> p t g e", g=4)
    msk = rt([128, 16, 4, 8]); P.tt(msk, lfa, ohg.us(3).bc((128, 16, 4, 8)), ALU.mult)
    lf = rt([128, 16, 8])
    P.tt(lf, msk[:, :, 0, :], msk[:, :, 1, :], ALU.add)
    P.tt(lf, lf, msk[:, :, 2, :], ALU.add); P.tt(lf, lf, msk[:, :, 3, :], ALU.add)
    top8 = rt([128, 16, 8])
    for t in range(16):
        P.add("dve", lambda e, t=t: e.max(out=top8.ap[:, t, :], in_=lf.ap[:, t, :]),
              reads=list(lf.keys), pwrites=list(top8.keys))
    m1 = top8[:, :, 0:1]; m2_ = top8[:, :, 1:2]
    ef = rt([128, 16, 8]); P.tt(ef, lf, m1.bc((128, 16, 8)), ALU.subtract)
    P.actf(ef, ef, AF.Exp)
    mk2 = rt([128, 16, 8]); P.tt(mk2, lf, m2_.bc((128, 16, 8)), ALU.subtract)
    P.ts(mk2, mk2, 1e30, ALU.mult, s2=1.0, op1=ALU.add)
    P.ts(mk2, mk2, 0.0, ALU.max, s2=1.0, op1=ALU.min)
    P.tt(ef, ef, mk2, ALU.mult)
    den2 = rt([128, 16]); reduce_x(den2, ef, ALU.add)
    rd2 = rt([128, 16]); P.recip(rd2, den2)
    P.tt(rd2, rd2, pg, ALU.mult)
    P.tt(ef, ef, rd2.us(2).bc((128, 16, 8)), ALU.mult)
    P.tt(comb, ohg.us(3).bc((128, 16, 4, 8)), ef.us(2).bc((128, 16, 4, 8)), ALU.mult)
    if "comb" in D:
        P.dma(D["comb"].rearrange("(t p) e -> p t e", p=128), comb.re("p t g e -> p t (g e)"))
    he = [P.tile([128, 2, 512], BF16, f"he{i}", es=es_m) for i in range(2)]
    gsb = [P.tile([128, 512], BF16, f"gsb{i}", es=es_m) for i in range(2)]
    es_ps = ExitStack()
    psG = [P.ptile([128, 512], F32, f"psG{i}", es=es_ps) for i in range(2)]
    psUu = [P.ptile([128, 512], F32, f"psUu{i}", es=es_ps) for i in range(2)]
    psDn = [P.ptile([128, 1024], F32, f"psDn{i}", es=es_ps) for i in range(2)]
    dn_cnt = [0]

    def down_tile(e, tb, tl):
        b = e % NB
        hb = he[tb % 2]
        tt_i = tb * 4 + tl
        pd = psDn[dn_cnt[0] % 2]; dn_cnt[0] += 1
        for hh in range(2):
            for ft in range(2):
                P.mm(pd[:, hh * 512:(hh + 1) * 512], hb[:, ft, tl * 128:(tl + 1) * 128],
                     Wd[b][:, ft, hh * 512:(hh + 1) * 512], ft == 0, ft == 1)
        hk = h_tm[:, tt_i, :].rk(("h", tt_i))
        P.stt(hk, pd, comb[:, tt_i, e // 8, (e % 8):(e % 8) + 1], hk, ALU.mult, ALU.add)

    for e in range(N_EXPERTS_RUN):
        b = e % NB
        for tb in range(4):
            if tb == 1 and e + NB - 1 < N_EXPERTS_RUN:
                load_expert(e + NB - 1)
            if tb == 2 and e == min(4, N_EXPERTS_RUN - 1):
                P.dma(Wpg, I["w_pg"].rearrange("(c p) f -> p c f", p=128), q="pool")
                P.dma(Wpp, I["w_pp"].rearrange("(c p) f -> p c f", p=128), q="pool")
            xk = xnT[:, :, tb * 512:(tb + 1) * 512].rk(("xnT2", tb))
            if tb >= 1:
                pend = [(e, tb - 1, tl) for tl in range(4)]
            elif e >= 1:
                pend = [(e - 1, 3, tl) for tl in range(4)]
            else:
                pend = []
            for ft in range(2):
                pg_ = psG[ft]; pu_ = psUu[ft]
                for dc in range(8):
                    P.mm(pg_, Wg[b][:, dc, ft * 128:(ft + 1) * 128], xk[:, dc, :], dc == 0, dc == 7)
                if pend:
                    down_tile(*pend.pop(0))
                for dc in range(8):
                    P.mm(pu_, Wu[b][:, dc, ft * 128:(ft + 1) * 128], xk[:, dc, :], dc == 0, dc == 7)
                P.actf(gsb[ft], pg_, AF.Silu)
                P.tt(he[tb % 2][:, ft, :], gsb[ft], pu_, ALU.mult, pw=True)
                if pend:
                    down_tile(*pend.pop(0))
    for tl in range(4):
        down_tile(N_EXPERTS_RUN - 1, 3, tl)
    P.barrier()
    es_ps.close()
    es_m.close()
    if "h2" in D:
        P.dma(D["h2"].rearrange("(t p) d -> p t d", p=128), h_tm)
        P.barrier()
    if STOP_AFTER == "moe":
        P.emit(final_wait_keys=["OUT"])
        return nc

    es_3 = ExitStack()
    load_gain(2)
    gbc2 = P.tile([128, 1024], F32, "gbc2", es=es_3)
    P.dma(gbc2, I["gvec"][3:4, :].partition_broadcast(128).rearrange("p a n -> p (a n)"))

    def ring3(n, shape, dt, name):
        return [P.tile(shape, dt, f"{name}{i}", es=es_3) for i in range(n)]

    pst = ring3(2, [128, 256], F32, "ptst"); pbf = ring3(2, [128, 256], BF16, "pbf")
    xh3 = ring3(2, [128, 1024], BF16, "xh3"); x3T = ring3(2, [128, 8, 128], BF16, "x3T")
    pTt = ring3(2, [128, 2, 128], BF16, "pTt")
    gts = ring3(2, [128, 1024], F32, "gts"); ost = ring3(2, [128, 1024], F32, "ost")
    sq3 = P.tile([128, 1024], BF16, "sq3", es=es_3)
    NS3 = 3
    ss3 = [ring3(NS3, [128, 1], F32, f"ss3{j}_") for j in range(2)]
    st3 = [ring3(NS3, [128, 1], F32, f"st3{j}_") for j in range(2)]
    sr3 = [ring3(NS3, [128, 1], F32, f"sr3{j}_") for j in range(2)]
    es_ps = ExitStack()
    psT3 = P.ptile([128, 8, 128], BF16, "psT3", es=es_ps)
    psP3 = P.ptile([128, 8, 128], BF16, "psP3", es=es_ps)
    psGt = [P.ptile([128, 1024], F32, f"psGt{i}", es=es_ps) for i in range(2)]
    psPp = P.ptile([128, 1024], F32, "psPp", es=es_ps)

    def hk_(t):
        return h_tm[:, t, :].rk(("h", t))

    def c_s0(t):
        P.dma(pst[t % 2], I["p"][t * 128:(t + 1) * 128, :])
        P.actf(sq3, hk_(t), AF.Square, accum=ss3[0][t % NS3])
        rstd_from_ss(ss3[0][t % NS3], 1024, st3[0][t % NS3], sr3[0][t % NS3])

    def c_s1(t):
        P.stt(xh3[t % 2], hk_(t), sr3[0][t % NS3], gbc, ALU.mult, ALU.mult)
        P.cp(pbf[t % 2], pst[t % 2], eng="act")
        for dc in range(8):
            P.tr(psT3[:, dc, :], xh3[t % 2][:, dc * 128:(dc + 1) * 128], identb)
        for kk in range(2):
            P.tr(psP3[:, kk, :], pbf[t % 2][:, kk * 128:(kk + 1) * 128], identb)
        P.cp(x3T[t % 2], psT3, eng="dve")
        P.cp(pTt[t % 2], psP3[:, 0:2, :], eng="act")

    def c_s2(t):
        for hh in range(2):
            for dc in range(8):
                P.mm(psGt[t % 2][:, hh * 512:(hh + 1) * 512], x3T[t % 2][:, dc, :], Wpg[:, dc, hh * 512:(hh + 1) * 512], dc == 0, dc == 7)
        for hh in range(2):
            for kk in range(2):
                P.mm(psPp[:, hh * 512:(hh + 1) * 512], pTt[t % 2][:, kk, :], Wpp[:, kk, hh * 512:(hh + 1) * 512], kk == 0, kk == 1)

    def c_s3(t):
        P.actf(gts[t % 2], psGt[t % 2], AF.Sigmoid)
        P.tt(gts[t % 2], gts[t % 2], psPp, ALU.mult)
        P.tt(hk_(t), hk_(t), gts[t % 2], ALU.add)

    def c_s4(t):
        P.actf(sq3, hk_(t), AF.Square, accum=ss3[1][t % NS3])
        rstd_from_ss(ss3[1][t % NS3], 1024, st3[1][t % NS3], sr3[1][t % NS3])

    def c_s5(t):
        P.stt(ost[t % 2], hk_(t), sr3[1][t % NS3], gbc2, ALU.mult, ALU.mult)
        P.dma(out_d[t * 128:(t + 1) * 128, :], ost[t % 2])

    pipeline(16, [c_s0, c_s1, c_s2, c_s3, c_s4, c_s5])
    P.barrier()
    es_ps.close()
    es_3.close()
    P.emit(final_wait_keys=["OUT"])
    return nc


def _host_consts():
    c = {}
    c["ident"] = np.eye(128, dtype=np.float32)
    s = np.arange(128)
    c["maskS"] = (s[:, None] <= s[None, :]).astype(np.float32)
    i_of = s // 16
    c["maskM"] = (i_of[None, :] >= i_of[:, None]).astype(np.float32)
    hm = np.zeros((128, 2), np.float32); hm[:64, 0] = 1; hm[64:, 1] = 1
    c["hmask"] = hm
    return c


def _pg(a):
    return np.ascontiguousarray(a.reshape(16, 2, 64).transpose(1, 2, 0).reshape(128, 16))


def _pgc(a):
    return np.ascontiguousarray(a.reshape(16, 2, 64, 16).transpose(1, 2, 0, 3).reshape(128, 256))


def make_in_maps(inputs):
    f = lambda k: np.asarray(inputs[k], dtype=np.float32)
    x = f("x"); p = f("p")[0]
    shared = _host_consts()
    shared["w_in"] = np.ascontiguousarray(f("w_in")[0]); shared["w_glu"] = np.ascontiguousarray(f("w_glu")[0])
    shared["w_out"] = np.ascontiguousarray(f("w_out")[0]); shared["w_pg"] = np.ascontiguousarray(f("w_ple_gate")[0])
    shared["w_pp"] = np.ascontiguousarray(f("w_ple_proj")[0])
    shared["wg"] = np.ascontiguousarray(f("w_gate_e")[0].reshape(32, 1024, 256))
    shared["wu"] = np.ascontiguousarray(f("w_up_e")[0].reshape(32, 1024, 256))
    shared["wd"] = np.ascontiguousarray(f("w_down_e")[0].reshape(32, 256, 1024))
    shared["wr"] = np.ascontiguousarray(np.concatenate([f("w_coarse")[0]] + [f("w_fine")[0, g] for g in range(4)], axis=1))
    shared["br"] = np.ascontiguousarray(np.concatenate([f("b_coarse")[0], f("b_fine")[0].reshape(-1)])[None, :])
    gv = np.zeros((6, 1024), np.float32)
    gv[0] = f("norm1")[0]; gv[1] = f("norm2")[0]; gv[2] = f("norm3")[0]; gv[3] = f("final_norm")
    gv[4, :512] = f("out_norm_a")[0]; gv[4, 512:] = f("out_norm_b")[0]
    gv[5, :512] = f("sgu_norm")[0]; gv[5, 512:] = f("d_skip")[0]
    shared["gvec"] = gv
    shared["gT"] = np.ascontiguousarray(f("norm1")[0].reshape(8, 128).T)
    bs = f("b_spatial")[0]
    shared["bsp"] = np.ascontiguousarray(np.repeat(bs.T[:, :, None], 64, axis=2).reshape(128, 512))
    shared["wsT"] = np.ascontiguousarray(f("w_spatial")[0].transpose(0, 2, 1))
    shared["are"] = _pg(f("a_re")[0]); shared["aim"] = _pg(f("a_im")[0])
    shared["ldt"] = _pg(np.repeat(f("log_dt")[0][:, None], 64, axis=1))
    shared["bre"] = _pgc(f("b_re")[0]); shared["bim"] = _pgc(f("b_im")[0])
    shared["cre"] = _pgc(f("c_re")[0].transpose(0, 2, 1)); shared["cim"] = _pgc(f("c_im")[0].transpose(0, 2, 1))
    shared["bglu"] = np.ascontiguousarray(f("b_glu"))
    in_maps = []
    for c in range(8):
        b, half = c // 2, c % 2
        m = dict(shared)
        m["x"] = np.ascontiguousarray(x[b, half * NTOK:(half + 1) * NTOK])
        m["xprev"] = np.ascontiguousarray(x[b, 0:NTOK]) if half == 1 else np.zeros((NTOK, 1024), np.float32)
        m["p"] = np.ascontiguousarray(p[b, half * NTOK:(half + 1) * NTOK])
        in_maps.append(m)
    return in_maps


def run(inputs, dbg_specs=None, cores=8):
    nc = bass.Bass("TRN2", target_bir_lowering=False)
    build_program(nc, dbg_specs or {})
    in_maps = make_in_maps(inputs)[:cores]
    res = run_bass_kernel_spmd(nc, in_maps, core_ids=list(range(cores)))
    return res.results


def kernel(**inputs):
    results = run(inputs)
    out = np.zeros((4, 4096, 1024), np.float32)
    for c in range(8):
        b, half = c // 2, c % 2
        out[b, half * NTOK:(half + 1) * NTOK] = results[c]["out"]
    return out
```

```python
import math
import numpy as np
import ml_dtypes
from contextlib import ExitStack
import concourse.bass as bass
import concourse.mybir as mybir
from concourse.bass_utils import run_bass_kernel_spmd

F32 = mybir.dt.float32
BF16 = mybir.dt.bfloat16
AF = mybir.ActivationFunctionType
ALU = mybir.AluOpType

SAME_ENG_WAIT = True
N_DMA_SEMS = 24
DEBUG = {}
STOP_AFTER = None
SUB = None
NO_HALO = False
N_EXPERTS_RUN = 32
EPS = 1e-6
NTOK = 2048
GELU = AF.Gelu_apprx_tanh


def _is_psum_key(k):
    if isinstance(k, tuple):
        k = k[0]
    return isinstance(k, str) and k.startswith("ps")


class Op:
    __slots__ = ("eng", "fn", "deps", "signal", "seq", "is_dma", "slot", "target", "idx")


class V:
    def __init__(self, ap, keys):
        self.ap = ap
        self.keys = tuple(keys) if isinstance(keys, (list, tuple)) else (keys,)

    def __getitem__(self, idx):
        return V(self.ap[idx], self.keys)

    def rk(self, *keys):
        return V(self.ap, keys)

    def re(self, pattern, **kw):
        return V(self.ap.rearrange(pattern, **kw), self.keys)

    def bc(self, shape):
        return V(self.ap.broadcast_to(list(shape)), self.keys)

    def us(self, d):
        return V(self.ap.unsqueeze(d), self.keys)


class Prog:
    def __init__(self, nc):
        self.nc = nc
        self.ops = []
        self.es = ExitStack()
        self.st = {}
        self.dma_uses = [0] * N_DMA_SEMS
        self.n_dma = 0
        self.n_dma_sw = 0
        self.uid = 0
        self.last = {}
        self.dmas_since_barrier = []

    def tile(self, shape, dtype, key=None, es=None):
        self.uid += 1
        name = f"t{self.uid}"
        t = (es or self.es).enter_context(self.nc.sbuf_tensor(name, list(shape), dtype))
        return V(t[:], key or name)

    def ptile(self, shape, dtype, key=None, es=None):
        self.uid += 1
        name = f"p{self.uid}"
        t = (es or self.es).enter_context(self.nc.psum_tensor(name, list(shape), dtype))
        return V(t[:], key or name)

    def _s(self, k):
        s = self.st.get(k)
        if s is None:
            s = self.st[k] = {"w": [], "r": [], "war": []}
        return s

    def add(self, eng, fn, reads=(), writes=(), pwrites=(), is_dma=False, extra_deps=()):
        op = Op()
        op.eng = eng
        op.fn = fn
        op.signal = False
        op.seq = 0
        op.is_dma = is_dma
        op.idx = len(self.ops)
        deps = set(extra_deps)
        for k in reads:
            s = self._s(k)
            deps.update(s["w"])
            if _is_psum_key(k):
                for r_ in s["r"]:
                    if self.ops[r_].eng != eng:
                        deps.add(r_)
            s["r"].append(op.idx)
        for k in writes:
            s = self._s(k)
            deps.update(s["r"])
            deps.update(s["w"])
            deps.update(s["war"])
            s["w"] = [op.idx]
            s["r"] = []
            s["war"] = []
        for k in pwrites:
            s = self._s(k)
            if s["r"]:
                s["war"] = list(s["r"]) + list(s["w"])
                s["r"] = []
                s["w"] = []
            deps.update(s["war"])
            s["w"].append(op.idx)
        deps.discard(op.idx)
        if is_dma:
            half = N_DMA_SEMS // 2
            if eng == "pool":
                op.slot = half + self.n_dma_sw % half
                self.n_dma_sw += 1
            else:
                op.slot = self.n_dma % half
                self.n_dma += 1
            self.dma_uses[op.slot] += 1
            op.target = 16 * self.dma_uses[op.slot]
            self.dmas_since_barrier.append(op.idx)
        elif fn is not None:
            self.last[eng] = op.idx
        op.deps = deps
        self.ops.append(op)
        return op

    def barrier(self):
        deps = set(self.last.values()) | set(self.dmas_since_barrier)
        self.dmas_since_barrier = []
        for e in ("pe", "act", "dve", "pool", "sp"):
            self.add(e, None, extra_deps=deps)
        self.st = {}

    def emit(self, final_wait_keys=()):
        nc = self.nc
        self.add("sp", None, reads=final_wait_keys)
        ops = self.ops

        def skip_same(dop, op):
            return dop.eng == op.eng and (not op.is_dma) and (dop.eng == "pe" or not SAME_ENG_WAIT)

        for op in ops:
            for d in op.deps:
                dop = ops[d]
                if dop.is_dma or skip_same(dop, op):
                    continue
                dop.signal = True
        cnt = {}
        for op in ops:
            if op.signal and not op.is_dma:
                cnt[op.eng] = cnt.get(op.eng, 0) + 1
                op.seq = cnt[op.eng]
        engs = ["pe", "act", "dve", "pool", "sp"]
        sems = {e: self.es.enter_context(nc.semaphore(f"sem_{e}")) for e in engs}
        dsem = [self.es.enter_context(nc.semaphore(f"dsem{i}")) for i in range(N_DMA_SEMS)]
        streams = {e: [op for op in ops if op.eng == e] for e in engs}

        def run(eng_name, eng):
            waited = {}
            for op in streams[eng_name]:
                need = {}
                for d in op.deps:
                    dop = ops[d]
                    if dop.is_dma:
                        key = ("d", dop.slot)
                        val = dop.target
                    else:
                        if skip_same(dop, op):
                            continue
                        key = ("e", dop.eng)
                        val = dop.seq
                    if val > need.get(key, 0):
                        need[key] = val
                if op.is_dma:
                    key = ("d", op.slot)
                    val = op.target - 16
                    if val > need.get(key, 0):
                        need[key] = val
                for key, val in need.items():
                    if waited.get(key, 0) >= val:
                        continue
                    waited[key] = val
                    sem = dsem[key[1]] if key[0] == "d" else sems[key[1]]
                    eng.wait_ge(sem, val)
                if op.fn is None:
                    continue
                ins = op.fn(eng)
                if op.is_dma:
                    ins.then_inc(dsem[op.slot], 16)
                elif op.signal:
                    ins.then_inc(sems[op.eng], 1)

        with nc.Block() as block:
            @block.tensor
            def _(e):
                run("pe", e)

            @block.scalar
            def _(e):
                run("act", e)

            @block.vector
            def _(e):
                run("dve", e)

            @block.gpsimd
            def _(e):
                run("pool", e)

            @block.sync
            def _(e):
                run("sp", e)
        self.es.close()

    def _rw(self, out, ins, pw):
        reads = []
        for x in ins:
            if isinstance(x, V):
                reads.extend(x.keys)
        if pw:
            return dict(reads=reads, pwrites=list(out.keys))
        return dict(reads=reads, writes=list(out.keys))

    @staticmethod
    def _a(x):
        return x.ap if isinstance(x, V) else x

    def tt(self, out, a, b, op, eng="dve", pw=False):
        self.add(eng, lambda e: e.tensor_tensor(out=out.ap, in0=a.ap, in1=b.ap, op=op), **self._rw(out, (a, b), pw))

    def ts(self, out, a, s1, op0, s2=None, op1=None, eng="dve", pw=False):
        kw = {}
        if op1 is not None:
            kw["op1"] = op1
        self.add(eng, lambda e: e.tensor_scalar(out=out.ap, in0=a.ap, scalar1=self._a(s1), scalar2=self._a(s2), op0=op0, **kw),
                 **self._rw(out, (a, s1, s2), pw))

    def stt(self, out, a, s, b, op0, op1, pw=False):
        self.add("dve", lambda e: e.scalar_tensor_tensor(out=out.ap, in0=a.ap, scalar=self._a(s), in1=b.ap, op0=op0, op1=op1),
                 **self._rw(out, (a, s, b), pw))

    def actf(self, out, a, func, scale=None, bias=None, accum=None, pw=False):
        kw = {}
        if scale is not None:
            kw["scale"] = self._a(scale)
        if bias is not None:
            kw["bias"] = self._a(bias)
        rw = self._rw(out, (a, scale, bias), pw)
        if accum is not None:
            kw["accum_out"] = accum.ap
            rw.setdefault("writes", [])
            rw["writes"] = list(rw["writes"]) + list(accum.keys)
        self.add("act", lambda e: e.activation(out=out.ap, in_=a.ap, func=func, **kw), **rw)

    def cp(self, out, a, eng="dve", pw=False):
        if eng == "act":
            self.add("act", lambda e: e.copy(out=out.ap, in_=a.ap), **self._rw(out, (a,), pw))
        else:
            self.add(eng, lambda e: e.tensor_copy(out=out.ap, in_=a.ap), **self._rw(out, (a,), pw))

    def mm(self, out, lhsT, rhs, start, stop):
        self.add("pe", lambda e: e.matmul(out.ap, lhsT=lhsT.ap, rhs=rhs.ap, start=start, stop=stop),
                 reads=list(lhsT.keys) + list(rhs.keys), pwrites=list(out.keys))

    def tr(self, out, a, ident):
        self.add("pe", lambda e: e.transpose(out=out.ap, in_=a.ap, identity=ident.ap),
                 reads=list(a.keys) + list(ident.keys), pwrites=list(out.keys))

    def scan(self, out, d0, d1, init):
        self.add("dve", lambda e: e.tensor_tensor_scan(out=out.ap, data0=d0.ap, data1=d1.ap, initial=self._a(init),
                                                        op0=ALU.mult, op1=ALU.add),
                 **self._rw(out, (d0, d1, init), True))

    def recip(self, out, a):
        self.add("dve", lambda e: e.reciprocal(out=out.ap, in_=a.ap), **self._rw(out, (a,), False))

    def dma(self, out, a, q="sp", pw=False, **kw):
        oap = self._a(out)
        iap = self._a(a)
        reads = list(a.keys) if isinstance(a, V) else []
        wk = list(out.keys) if isinstance(out, V) else ["OUT"]
        if pw or not isinstance(out, V):
            self.add(q, lambda e: e.dma_start(out=oap, in_=iap, **kw), reads=reads, pwrites=wk, is_dma=True)
        else:
            self.add(q, lambda e: e.dma_start(out=oap, in_=iap, **kw), reads=reads, writes=wk, is_dma=True)


def pipeline(nt, stages):
    ns = len(stages)
    for step in range(nt + ns - 1):
        for s_ in reversed(range(ns)):
            t = step - s_
            if 0 <= t < nt:
                stages[s_](t)


def build_program(nc, dbg_specs):
    P = Prog(nc)
    I = {}

    def din(name, shape, dt=F32):
        I[name] = nc.dram_tensor(name, list(shape), dt, kind="ExternalInput").ap()

    din("x", [NTOK, 1024]); din("xprev", [NTOK, 1024]); din("p", [NTOK, 256])
    din("w_in", [1024, 1536]); din("w_glu", [512, 1024]); din("w_out", [1024, 1024])
    din("w_pg", [1024, 1024]); din("w_pp", [256, 1024])
    din("wg", [32, 1024, 256]); din("wu", [32, 1024, 256]); din("wd", [32, 256, 1024])
    din("wr", [1024, 36]); din("br", [1, 36])
    din("gvec", [6, 1024])
    din("bsp", [128, 512]); din("wsT", [8, 128, 128]); din("maskS", [128, 128]); din("gT", [128, 8])
    din("are", [128, 16]); din("aim", [128, 16]); din("ldt", [128, 16])
    din("bre", [128, 256]); din("bim", [128, 256]); din("cre", [128, 256]); din("cim", [128, 256])
    din("maskM", [128, 128]); din("hmask", [128, 2])
    din("bglu", [1, 1024]); din("ident", [128, 128])
    out_d = nc.dram_tensor("out", [NTOK, 1024], F32, kind="ExternalOutput").ap()
    D = {}
    for name, shape in dbg_specs.items():
        D[name] = nc.dram_tensor(name, list(shape), F32, kind="ExternalOutput").ap()

    def dump(name, v, shape2d=None):
        if name in D:
            P.dma(D[name], v)

    ident = P.tile([128, 128], F32, "ident")
    identb = P.tile([128, 128], BF16, "identb")
    P.dma(ident, I["ident"])
    P.cp(identb, ident)
    gbc = P.tile([128, 1024], F32, "gbc")
    mixTb = P.tile([128, 4, NTOK], BF16, "mixTb")

    def load_gain(row, n=1024, col0=0):
        P.dma(gbc[:, 0:n], I["gvec"][row:row + 1, col0:col0 + n].partition_broadcast(128).rearrange("p a n -> p (a n)"))

    nhalf = P.tile([128, 1], F32, "nhalf")
    P.add("dve", lambda e: e.memset(nhalf.ap, -0.5), writes=["nhalf"])

    def rstd_from_ss(ss, n, tmp, out):
        P.ts(tmp, ss, 1.0 / n, ALU.mult, s2=EPS, op1=ALU.add, eng="pool")
        P.tt(out, tmp, nhalf, ALU.pow, eng="pool")

    def norm_tm(src, dst_bf, scratch, ss, tmp, rstd, lo_bf=None, xf=None):
        P.actf(scratch, src, AF.Square, accum=ss)
        rstd_from_ss(ss, 1024, tmp, rstd)
        if lo_bf is None:
            P.stt(dst_bf, src, rstd, gbc, ALU.mult, ALU.mult)
        else:
            P.stt(xf, src, rstd, gbc, ALU.mult, ALU.mult)
            P.cp(dst_bf, xf, eng="act")
            P.tt(lo_bf, xf, dst_bf, ALU.subtract)

    es_p1 = ExitStack()
    es_a = ExitStack()
    sm = lambda shape, key=None, dt=F32: P.tile(shape, dt, key, es=es_a)

    NSEG = 64
    NCH = 128
    rho = P.tile([128, 16], F32, "rho", es=es_a)
    Rr = P.tile([128, 16, NSEG], F32, "Rr", es=es_a); Ri = P.tile([128, 16, NSEG], F32, "Ri", es=es_a)
    QTp = P.tile([128, 32, 2, 128], BF16, "QTp", es=es_a)
    MT = P.tile([128, 32, 128], BF16, "MT", es=es_a)
    PT = P.tile([128, 16, 2, 128], BF16, "PT", es=es_a)
    Wins = P.tile([128, 8, 512], BF16, "Wins", es=es_a)
    P.dma(Wins, I["w_in"][:, 1024:1536].rearrange("(c p) f -> p c f", p=128), q="pool")
    gT1 = P.tile([128, 8], F32, "gT1", es=es_a); P.dma(gT1, I["gT"])
    es_s = ExitStack()
    sm = lambda shape, key=None, dt=F32: P.tile(shape, dt, key, es=es_s)
    hmask = sm([128, 2], "hmask"); P.dma(hmask, I["hmask"])
    are = sm([128, 16]); aim = sm([128, 16]); ldt = sm([128, 16])
    P.dma(are, I["are"]); P.dma(aim, I["aim"]); P.dma(ldt, I["ldt"])
    bre = sm([128, 16, 16]); bim = sm([128, 16, 16]); cre = sm([128, 16, 16]); cim = sm([128, 16, 16])
    for t, n in ((bre, "bre"), (bim, "bim"), (cre, "cre"), (cim, "cim")):
        P.dma(t.re("p a b -> p (a b)"), I[n])
    maskM = sm([128, 128]); P.dma(maskM, I["maskM"])
    ctmp = [sm([128, 2048]) for _ in range(4)]

    def view(t, shape):
        n = 1
        for d in shape[1:]:
            n *= d
        v = t[:, 0:n]
        if len(shape) == 3:
            return v.re("p (a b) -> p a b", a=shape[1])
        if len(shape) == 4:
            return v.re("p (a b c) -> p a b c", a=shape[1], b=shape[2])
        return v

    def newt(shape=(128, 16)):
        return sm(list(shape))

    def cmul(orr, oi, ar, ai, br_, bi_, shape):
        t1, t2, t3, t4 = [view(t, shape) for t in ctmp]
        P.tt(t1, ar, br_, ALU.mult); P.tt(t2, ai, bi_, ALU.mult)
        P.tt(t3, ar, bi_, ALU.mult); P.tt(t4, ai, br_, ALU.mult)
        P.tt(orr, t1, t2, ALU.subtract, pw=True); P.tt(oi, t3, t4, ALU.add, pw=True)

    MAGIC = 12582912.0
    kf0 = newt(); P.ts(kf0, ldt, 1.0 / math.log(2.0), ALU.mult)
    kf1 = newt(); P.ts(kf1, kf0, MAGIC, ALU.add)
    kf = newt(); P.ts(kf, kf1, -MAGIC, ALU.add)
    LN2_HI = 0.693359375; LN2_LO = -2.12194440e-4
    r0 = newt(); P.stt(r0, kf, -LN2_HI, ldt, ALU.mult, ALU.add)
    r1 = newt(); P.stt(r1, kf, -LN2_LO, r0, ALU.mult, ALU.add)
    ecoef = [1.0 / math.factorial(k_) for k_ in range(0, 12)]
    acc = newt(); P.ts(acc, r1, ecoef[-1], ALU.mult)
    for c_ in reversed(ecoef[1:-1]):
        nxt = newt(); P.stt(nxt, acc, c_, r1, ALU.add, ALU.mult); acc = nxt
    dt_ = newt(); P.ts(dt_, acc, 1.0, ALU.add)
    mneg = newt(); P.ts(mneg, kf, -1.0, ALU.mult)
    for bit in (16, 8, 4, 2, 1):
        bsel = newt(); P.ts(bsel, mneg, float(1 - bit), ALU.add, s2=0.0, op1=ALU.max)
        bb = newt(); P.ts(bb, bsel, 1.0, ALU.min)
        m2_ = newt(); P.stt(m2_, bb, -float(bit), mneg, ALU.mult, ALU.add); mneg = m2_
        fac = newt(); P.ts(fac, bb, 2.0 ** (-bit) - 1.0, ALU.mult, s2=1.0, op1=ALU.add)
        nd = newt(); P.tt(nd, dt_, fac, ALU.mult); dt_ = nd
    zr = newt(); zi = newt()
    P.tt(zr, are, dt_, ALU.mult); P.tt(zi, aim, dt_, ALU.mult)
    e0 = newt(); P.actf(e0, zr, AF.Exp, scale=1.0 / 16)
    xs = newt(); P.ts(xs, zi, 1.0 / 16, ALU.mult)
    u2 = newt(); P.tt(u2, xs, xs, ALU.mult)

    def horner(coefs, u):
        acc = newt(); P.ts(acc, u, coefs[-1], ALU.mult)
        for c in reversed(coefs[1:-1]):
            nxt = newt(); P.stt(nxt, acc, c, u, ALU.add, ALU.mult); acc = nxt
        return acc

    sc = [1.0] + [(-1.0) ** k / math.factorial(2 * k + 1) for k in range(1, 7)]
    cc = [1.0] + [(-1.0) ** k / math.factorial(2 * k) for k in range(1, 8)]
    ps_ = horner(sc, u2)
    sn = newt(); P.stt(sn, ps_, 1.0, xs, ALU.add, ALU.mult)
    pc_ = horner(cc, u2)
    cs = newt(); P.ts(cs, pc_, 1.0, ALU.add)
    wr_ = newt(); wi_ = newt()
    P.tt(wr_, e0, cs, ALU.mult); P.tt(wi_, e0, sn, ALU.mult)
    pw_hist = []
    for _ in range(4):
        nr = newt(); ni = newt(); t1 = newt(); t2 = newt()
        P.tt(t1, wr_, wr_, ALU.mult); P.tt(t2, wi_, wi_, ALU.mult); P.tt(nr, t1, t2, ALU.subtract)
        P.tt(t1, wr_, wi_, ALU.mult); P.ts(ni, t1, 2.0, ALU.mult)
        wr_, wi_ = nr, ni
    a_r, a_i = wr_, wi_
    nrr = newt(); P.ts(nrr, a_r, -1.0, ALU.add)
    den = newt(); t1 = newt(); t2 = newt()
    P.tt(t1, are, are, ALU.mult); P.tt(t2, aim, aim, ALU.mult); P.tt(den, t1, t2, ALU.add)
    rden = newt(); P.recip(rden, den)
    kr = newt(); ki = newt(); t3 = newt(); t4 = newt()
    P.tt(t1, nrr, are, ALU.mult); P.tt(t2, a_i, aim, ALU.mult); P.tt(t3, t1, t2, ALU.add); P.tt(kr, t3, rden, ALU.mult)
    P.tt(t1, a_i, are, ALU.mult); P.tt(t2, nrr, aim, ALU.mult); P.tt(t4, t1, t2, ALU.subtract); P.tt(ki, t4, rden, ALU.mult)
    S3 = (128, 16, 16)
    Bbr = newt(S3); Bbi = newt(S3)
    cmul(Bbr, Bbi, kr.us(2).bc(S3), ki.us(2).bc(S3), bre, bim, S3)
    pwr = newt((128, 16, 8)); pwi = newt((128, 16, 8))
    P.cp(pwr[:, :, 0:1], a_r.us(2), pw=True); P.cp(pwi[:, :, 0:1], a_i.us(2), pw=True)
    n = 1
    while n < 8:
        sh = (128, 16, n)
        cmul(pwr[:, :, n:2 * n], pwi[:, :, n:2 * n], pwr[:, :, 0:n], pwi[:, :, 0:n],
             pwr[:, :, n - 1:n].bc(sh), pwi[:, :, n - 1:n].bc(sh), sh)
        n *= 2
    rvr = newt((128, 16, 8)); rvi = newt((128, 16, 8))
    P.add("dve", lambda e: e.memset(rvr.ap[:, :, 7:8], 1.0), pwrites=list(rvr.keys))
    P.add("dve", lambda e: e.memset(rvi.ap[:, :, 7:8], 0.0), pwrites=list(rvi.keys))
    for i in range(7):
        P.cp(rvr[:, :, i:i + 1], pwr[:, :, 6 - i:7 - i], pw=True)
        P.cp(rvi[:, :, i:i + 1], pwi[:, :, 6 - i:7 - i], pw=True)
    A8r = pwr[:, :, 7]; A8i = pwi[:, :, 7]
    m2 = newt(); P.tt(t1, A8r, A8r, ALU.mult); P.tt(t2, A8i, A8i, ALU.mult); P.tt(m2, t1, t2, ALU.add)
    rm = newt(); P.recip(rm, m2)
    ivr = newt(); ivi = newt()
    P.tt(ivr, A8r, rm, ALU.mult); P.tt(t1, A8i, rm, ALU.mult); P.ts(ivi, t1, -1.0, ALU.mult)
    P.actf(rho, m2, AF.Sqrt)
    rrho = newt(); P.recip(rrho, rho)
    P.tt(Rr[:, :, 0:1], A8r.us(2), rrho.us(2), ALU.mult, pw=True)
    P.tt(Ri[:, :, 0:1], A8i.us(2), rrho.us(2), ALU.mult, pw=True)
    n = 1
    while n < NSEG:
        sh = (128, 16, n)
        cmul(Rr[:, :, n:2 * n], Ri[:, :, n:2 * n], Rr[:, :, 0:n], Ri[:, :, 0:n],
             Rr[:, :, n - 1:n].bc(sh), Ri[:, :, n - 1:n].bc(sh), sh)
        n *= 2
    S4 = (128, 16, 8, 16)
    PNr = newt(S4); PNi = newt(S4)
    cmul(PNr, PNi, rvr.us(3).bc(S4), rvi.us(3).bc(S4), Bbr.us(2).bc(S4), Bbi.us(2).bc(S4), S4)
    QNr = newt(S4); QNi = newt(S4)
    cmul(QNr, QNi, pwr.us(3).bc(S4), pwi.us(3).bc(S4), cre.us(2).bc(S4), cim.us(2).bc(S4), S4)
    QMr = newt(S4); QMi = newt(S4)
    cmul(QMr, QMi, QNr, QNi, ivr.us(2).us(3).bc(S4), ivi.us(2).us(3).bc(S4), S4)
    QMp = newt((128, 32, 2, 128))
    QTv = QTp.re("p (gp h) r c -> p gp h r c", h=2)
    QMv = QMp.re("p (gp h) r c -> p gp h r c", h=2)
    for h in range(2):
        hm = hmask[:, h:h + 1]
        P.ts(QTv[:, :, h, 0, :], QNr.re("p g j c -> p g (j c)"), hm, ALU.mult, pw=True)
        P.ts(QTv[:, :, h, 1, :], QNi.re("p g j c -> p g (j c)"), hm, ALU.mult, s2=-1.0, op1=ALU.mult, pw=True)
        P.ts(QMv[:, :, h, 0, :], QMr.re("p g j c -> p g (j c)"), hm, ALU.mult, pw=True)
        P.ts(QMv[:, :, h, 1, :], QMi.re("p g j c -> p g (j c)"), hm, ALU.mult, s2=-1.0, op1=ALU.mult, pw=True)
    with ExitStack() as es_sp:
        ps_m = [P.ptile([128, 4, 128], F32, f"ps_m{i}", es=es_sp) for i in range(2)]
        ps_t = [P.ptile([128, 4, 128], F32, f"ps_t{i}", es=es_sp) for i in range(2)]
        for q4 in range(8):
            pm = ps_m[q4 % 2]
            for k_ in range(4):
                g = q4 * 4 + k_
                gp = g // 2
                P.mm(pm[:, k_, :], PNr[:, gp].re("p i c -> p (i c)"), QMp[:, g, 0, :], True, False)
                P.mm(pm[:, k_, :], PNi[:, gp].re("p i c -> p (i c)"), QMp[:, g, 1, :], False, True)
            P.tt(MT[:, q4 * 4:(q4 + 1) * 4, :], pm, maskM.us(1).bc((128, 4, 128)), ALU.mult, pw=True)
        for q2 in range(8):
            pt_ = ps_t[q2 % 2]
            for k_ in range(2):
                gp = q2 * 2 + k_
                for ri, pn in ((0, PNr), (1, PNi)):
                    P.tr(pt_[:, 2 * k_ + ri, :], pn[:, gp].re("p i c -> p (i c)"), ident)
            P.cp(PT[:, q2 * 2:(q2 + 1) * 2].re("p a b c -> p (a b) c"), pt_, eng="act", pw=True)
        P.barrier()
    def dump_bf(name, v2d, ncols):
        if name not in D:
            return
        for i, c0 in enumerate(range(0, ncols, 2048)):
            n_ = min(2048, ncols - c0)
            tmp = ctmp[i % 4][:, 0:n_]
            P.cp(tmp, v2d[:, c0:c0 + n_])
            P.dma(D[name][:, c0:c0 + n_], tmp)

    dump_bf("MT", MT.re("p g c -> p (g c)"), 32 * 128)
    dump_bf("PT", PT.re("p a b c -> p (a b c)"), 16 * 2 * 128)
    dump_bf("QT", QTp.re("p a b c -> p (a b c)"), 32 * 2 * 128)
    if "Rr" in D:
        P.dma(D["Rr"], Rr.re("p a b -> p (a b)")); P.dma(D["Ri"], Ri.re("p a b -> p (a b)")); P.dma(D["rho"], rho)
    P.barrier()
    es_s.close()
    if STOP_AFTER == "setup":
        es_a.close(); es_p1.close()
        P.emit(final_wait_keys=["OUT"])
        return nc


    Wglu = P.tile([128, 4, 1024], BF16, "Wglu", es=es_a)
    P.dma(Wglu, I["w_glu"].rearrange("(c p) f -> p c f", p=128), q="pool")
    bglu_bc = P.tile([128, 1024], F32, "bglu_bc", es=es_a)
    P.dma(bglu_bc, I["bglu"].partition_broadcast(128).rearrange("p a n -> p (a n)"))
    dsk_bc = P.tile([128, 512], F32, "dsk_bc", es=es_a)
    P.dma(dsk_bc, I["gvec"][5:6, 512:1024].partition_broadcast(128).rearrange("p a n -> p (a n)"))
    gob_bc = P.tile([128, 512], F32, "gob_bc", es=es_a)
    P.dma(gob_bc, I["gvec"][4:5, 512:1024].partition_broadcast(128).rearrange("p a n -> p (a n)"))
    load_gain(0)
    xT_blk = P.tile([128, 8, 1024], BF16, "xTblk", es=es_a)
    Zb2 = [P.tile([128, 32, 8, 16], BF16, f"Zb{i}", es=es_a) for i in range(2)]
    Ub2 = [P.tile([128, 32, 128], BF16, f"Ub{i}", es=es_a) for i in range(2)]
    T1 = P.tile([128, 8, NSEG], F32, "T1", es=es_a); T2 = P.tile([128, 8, NSEG], F32, "T2", es=es_a)
    TA = P.tile([128, 8, NSEG], F32, "TA", es=es_a); TB = P.tile([128, 8, NSEG], F32, "TB", es=es_a)
    Xr2 = [P.tile([128, 16, NCH + 1], BF16, f"Xr{i}", es=es_a) for i in range(2)]
    Xi2 = [P.tile([128, 16, NCH + 1], BF16, f"Xi{i}", es=es_a) for i in range(2)]
    xin_r = P.tile([128, 16], F32, "xin_r", es=es_a); xin_i = P.tile([128, 16], F32, "xin_i", es=es_a)
    P.add("dve", lambda e: e.memset(xin_r.ap, 0.0), writes=["xin_r"])
    P.add("dve", lambda e: e.memset(xin_i.ap, 0.0), writes=["xin_i"])
    ds2 = [P.tile([128, 8, 8, 16], BF16, f"ds_bf{i}", es=es_a) for i in range(2)]
    ypre2 = [P.tile([128, 8, 128], F32, f"ypre{i}", es=es_a) for i in range(2)]
    y_bf = P.tile([128, 8, 512], BF16, "y_bf", es=es_a)
    obn = y_bf
    xst = [P.tile([128, 1024], F32, f"xst{i}", es=es_a) for i in range(2)]
    xnb = [P.tile([128, 1024], BF16, f"xnb{i}", es=es_a) for i in range(4)]
    sq_scr = P.tile([128, 1024], BF16, "sq_scr", es=es_a)
    gas = [P.tile([128, 512], F32, f"ga{i}", es=es_a) for i in range(3)]
    gss = [P.tile([128, 512], F32, f"gs{i}", es=es_a) for i in range(3)]
    sst3 = [P.tile([128, 1], F32, f"ssg{i}", es=es_a) for i in range(3)]
    stmp3 = [P.tile([128, 1], F32, f"stg{i}", es=es_a) for i in range(3)]
    srs3 = [P.tile([128, 1], F32, f"srg{i}", es=es_a) for i in range(3)]
    sst = [P.tile([128, 1], F32, f"ss{i}", es=es_a) for i in range(2)]
    stmp = [P.tile([128, 1], F32, f"stmp{i}", es=es_a) for i in range(2)]
    srs = [P.tile([128, 1], F32, f"srs{i}", es=es_a) for i in range(2)]
    cr1 = P.tile([128, 8], F32, "cr1", es=es_a); cr2 = P.tile([128, 8], F32, "cr2", es=es_a)

    es_ps = ExitStack()
    psN = P.ptile([128, 4, 512], BF16, "psN", es=es_ps)
    psZ = [P.ptile([128, 512], F32, f"psZa{i}", es=es_ps) for i in range(2)]
    psA = P.ptile([128, 2, 512], F32, "psA", es=es_ps)
    psB = P.ptile([128, 2, 512], F32, "psB", es=es_ps)
    psAf = psA.re("p a b -> p (a b)"); psBf = psB.re("p a b -> p (a b)")

    def psN_q(q):
        return psN[:, q, :].rk(("psN", q // 2))

    def psN_bank(b_):
        return psN[:, 2 * b_:2 * b_ + 2, :].re("p a b -> p (a b)").rk(("psN", b_))

    def A_front(blk):
        main = blk >= 2
        src = I["x"] if main else I["xprev"]
        t0 = (blk % 2) * 1024
        zb = Zb2[blk % 2]; ub = Ub2[blk % 2]
        for half in range(2):
            for tl in range(4):
                tt_i = half * 4 + tl
                xs_ = xst[tt_i % 2]
                P.dma(xs_, src[t0 + tt_i * 128: t0 + (tt_i + 1) * 128, :])
                k = tt_i % 2
                P.actf(sq_scr, xs_, AF.Square, accum=sst[k])
                rstd_from_ss(sst[k], 1024, stmp[k], srs[k])
                P.actf(xnb[tl], xs_, AF.Copy, scale=srs[k])
            for rnd in range(2):
                for dq in range(4):
                    dc = rnd * 4 + dq
                    for tl in range(4):
                        P.tr(psN_q(dq)[:, tl * 128:(tl + 1) * 128], xnb[tl][:, dc * 128:(dc + 1) * 128], identb)
                for dq in range(4):
                    dc = rnd * 4 + dq
                    if blk < 2 and dq % 2 == 1:
                        P.ts(xT_blk[:, dc, half * 512:(half + 1) * 512], psN_q(dq), gT1[:, dc:dc + 1], ALU.mult, pw=True)
                    else:
                        P.actf(xT_blk[:, dc, half * 512:(half + 1) * 512], psN_q(dq), AF.Copy, scale=gT1[:, dc:dc + 1], pw=True)
        for i in range(8):
            pz = psZ[i % 2]
            for dc in range(8):
                lhs = xT_blk[:, dc, :].re("p (c e) -> p c e", e=8)[:, :, i]
                P.mm(pz, lhs, Wins[:, dc, :], dc == 0, dc == 7)
            P.cp(zb[:, :, i, :], pz.re("p (g c) -> p g c", g=32), eng=("dve" if (blk == 0 or (blk == 1 and i % 2)) else "act"), pw=True)
        for g8 in range(4):
            pu2 = psN_bank(g8 % 2)
            for gl in range(8):
                g = g8 * 8 + gl
                P.tr(pu2[:, gl * 128:(gl + 1) * 128], zb[:, g].re("p i c -> p (i c)"), identb)
            P.cp(ub[:, g8 * 8:(g8 + 1) * 8, :].re("p a b -> p (a b)"), pu2, eng=("dve" if (blk == 0 or (blk == 1 and g8 % 2)) else "act"), pw=True)

    def A_level1(blk, halves=(0, 1)):
        main = blk >= 2
        ub = Ub2[blk % 2]
        Xr = Xr2[blk % 2]; Xi = Xi2[blk % 2]
        for hf in halves:
            gsl = slice(hf * 8, (hf + 1) * 8)
            for gq in range(8):
                gp = hf * 8 + gq
                for h in range(2):
                    g = 2 * gp + h
                    for ri, pp_ in ((0, psAf), (1, psBf)):
                        P.mm(pp_[64 * h:64 * h + 64, gq * 128:(gq + 1) * 128], PT[:, gp, ri, 64 * h:64 * h + 64], ub[:, g, :], True, True)
            if main:
                P.cp(Xr[:, gsl, 0:1], xin_r[:, gsl].us(2), pw=True); P.cp(Xi[:, gsl, 0:1], xin_i[:, gsl].us(2), pw=True)
            for seg in range(NCH // NSEG):
                csl = slice(seg * NSEG, (seg + 1) * NSEG)
                VrP = psAf.re("p (a b) -> p a b", a=8)[:, :, csl]
                ViP = psBf.re("p (a b) -> p a b", a=8)[:, :, csl]
                rr = Rr[:, gsl, :]; ri_ = Ri[:, gsl, :]
                P.tt(T1, rr, VrP, ALU.mult); P.tt(T2, rr, ViP, ALU.mult)
                P.tt(TA, ri_, ViP, ALU.mult); P.tt(TB, ri_, VrP, ALU.mult)
                P.tt(T1, T1, TA, ALU.add)
                P.tt(T2, T2, TB, ALU.subtract)
                for gq in range(8):
                    gp = hf * 8 + gq
                    rb = rho[:, gp:gp + 1].bc((128, NSEG))
                    P.scan(T1[:, gq, :], rb, T1[:, gq, :], xin_r[:, gp:gp + 1])
                    P.scan(T2[:, gq, :], rb, T2[:, gq, :], xin_i[:, gp:gp + 1])
                L = NSEG - 1
                P.tt(cr1, rr[:, :, L], T1[:, :, L], ALU.mult); P.tt(cr2, ri_[:, :, L], T2[:, :, L], ALU.mult)
                P.tt(xin_r[:, gsl], cr1, cr2, ALU.subtract, pw=True)
                P.tt(cr1, rr[:, :, L], T2[:, :, L], ALU.mult); P.tt(cr2, ri_[:, :, L], T1[:, :, L], ALU.mult)
                P.tt(xin_i[:, gsl], cr1, cr2, ALU.add, pw=True)
                if main:
                    xs0 = 1 + seg * NSEG
                    P.tt(TA, rr, T1, ALU.mult); P.tt(TB, ri_, T2, ALU.mult, eng="pool")
                    P.tt(Xr[:, gsl, xs0:xs0 + NSEG], TA, TB, ALU.subtract, pw=True)
                    P.tt(TA, rr, T2, ALU.mult); P.tt(TB, ri_, T1, ALU.mult, eng="pool")
                    P.tt(Xi[:, gsl, xs0:xs0 + NSEG], TA, TB, ALU.add, pw=True)

    def A_out(blk, part):
        t0 = (blk % 2) * 1024
        zb = Zb2[blk % 2]; ub = Ub2[blk % 2]
        Xr = Xr2[blk % 2]; Xi = Xi2[blk % 2]
        yT = ub.re("p (ct j) c -> p ct j c", ct=4)
        if part == 0:
            for ct in range(4):
                def pY(hh, ct=ct):
                    return psZ[hh] if ct % 2 == 0 else (psAf if hh == 0 else psBf)[:, 0:512]
                ds_ = ds2[ct % 2]; yp_ = ypre2[ct % 2]
                for gl in range(8):
                    g = ct * 8 + gl
                    gp = g // 2
                    o = pY(gl // 4)[:, (gl % 4) * 128:(gl % 4 + 1) * 128]
                    P.mm(o, Xr[:, gp, 0:NCH], QTp[:, g, 0, :], True, False)
                    P.mm(o, Xi[:, gp, 0:NCH], QTp[:, g, 1, :], False, False)
                    P.mm(o, ub[:, g, :], MT[:, g, :], False, True)
                P.tt(ds_, zb[:, ct * 8:(ct + 1) * 8], dsk_bc[:, ct * 128:(ct + 1) * 128].re("p (g c) -> p g c", g=8).us(2).bc((128, 8, 8, 16)), ALU.mult, eng="pool")
                ypv = yp_.re("p j (g c) -> p g j c", g=8)
                for hh in range(2):
                    yv = pY(hh).re("p (g j c) -> p g j c", g=4, j=8)
                    P.tt(ypv[:, hh * 4:(hh + 1) * 4], yv, ds_[:, hh * 4:(hh + 1) * 4], ALU.add, pw=True)
                P.actf(y_bf[:, :, ct * 128:(ct + 1) * 128], yp_, GELU, pw=True)
            return
        for ct in range(4):
            pq = psN_bank(ct % 2)
            for j in range(8):
                P.tr(pq[:, j * 128:(j + 1) * 128], y_bf[:, j, ct * 128:(ct + 1) * 128], identb)
            P.cp(yT[:, ct].re("p j c -> p (j c)"), pq, eng="act", pw=True)
        def pglu(j, hh):
            return (psZ[hh] if j % 2 == 0 else (psA if hh == 0 else psB)[:, 0, :])

        def g_s0(j):
            for hh in range(2):
                for ct in range(4):
                    P.mm(pglu(j, hh), yT[:, ct, j, :], Wglu[:, ct, hh * 512:(hh + 1) * 512], ct == 0, ct == 3)

        def g_s1(j):
            P.tt(gas[j % 3], pglu(j, 0), bglu_bc[:, 0:512], ALU.add)
            P.tt(gss[j % 3], pglu(j, 1), bglu_bc[:, 512:1024], ALU.add)
            P.actf(gss[j % 3], gss[j % 3], AF.Sigmoid)

        def g_s2(j):
            P.tt(gas[j % 3], gas[j % 3], gss[j % 3], ALU.mult)
            P.actf(sq_scr[:, 0:512], gas[j % 3], AF.Square, accum=sst3[j % 3])
            rstd_from_ss(sst3[j % 3], 512, stmp3[j % 3], srs3[j % 3])

        def g_s3(j):
            P.stt(obn[:, j, :], gas[j % 3], srs3[j % 3], gob_bc, ALU.mult, ALU.mult, pw=True)

        pipeline(8, [g_s0, g_s1, g_s2, g_s3])
        for ct in range(4):
            pq = psN_bank(ct % 2)
            for j in range(8):
                P.tr(pq[:, j * 128:(j + 1) * 128], obn[:, j, ct * 128:(ct + 1) * 128], identb)
            dst = mixTb[:, ct, t0:t0 + 1024].re("p (c j) -> p j c", j=8)
            P.cp(dst, pq.re("p (j c) -> p j c", j=8), eng="act", pw=True)

    if not NO_HALO:
        A_front(0)
        A_front(1)
        A_level1(0)
    A_front(2)
    if not NO_HALO:
        A_level1(1)
    A_front(3)
    A_level1(2)
    A_level1(3, halves=(0,))
    A_out(2, 0)
    A_level1(3, halves=(1,))
    A_out(2, 1)
    A_out(3, 0)
    A_out(3, 1)
    P.barrier()
    es_ps.close()
    if "mixTb" in D:
        mf = xst[0]
        for ct in range(4):
            for hh in range(2):
                P.cp(mf, mixTb[:, ct, hh * 1024:(hh + 1) * 1024])
                P.dma(D["mixTb"][:, ct * NTOK + hh * 1024: ct * NTOK + (hh + 1) * 1024], mf)
        P.barrier()
    es_a.close()
    if STOP_AFTER == "A":
        es_p1.close()
        P.emit(final_wait_keys=["OUT"])
        return nc

    h_tm = P.tile([128, 16, 1024], F32, "h")
    es_b = ExitStack()
    Winuv = P.tile([128, 8, 1024], BF16, "Winuv", es=es_b)
    Winu = Winuv[:, :, 0:512].rk("Winu"); Winv = Winuv[:, :, 512:1024].rk("Winv")
    P.dma(Winu, I["w_in"][:, 0:512].rearrange("(c p) f -> p c f", p=128), q="pool")
    P.dma(Winv, I["w_in"][:, 512:1024].rearrange("(c p) f -> p c f", p=128), q="pool")
    Wout = P.tile([128, 8, 1024], BF16, "Wout", es=es_b)
    P.dma(Wout, I["w_out"].rearrange("(c p) f -> p c f", p=128), q="pool")
    bsp = P.tile([128, 512], F32, "bsp", es=es_b); P.dma(bsp, I["bsp"])
    sgu_bc = P.tile([128, 512], F32, "sgu_bc", es=es_b)
    P.dma(sgu_bc, I["gvec"][5:6, 0:512].partition_broadcast(128).rearrange("p a n -> p (a n)"))
    goa_bc = P.tile([128, 512], F32, "goa_bc", es=es_b)
    P.dma(goa_bc, I["gvec"][4:5, 0:512].partition_broadcast(128).rearrange("p a n -> p (a n)"))
    wsTf = P.tile([128, 8, 128], F32, "wsTf", es=es_b)
    P.dma(wsTf, I["wsT"].rearrange("h s t -> s h t"))
    maskS = P.tile([128, 128], F32, "maskS", es=es_b); P.dma(maskS, I["maskS"])
    wsTb = P.tile([128, 8, 128], BF16, "wsTb", es=es_b)
    P.tt(wsTb, wsTf, maskS.us(1).bc((128, 8, 128)), ALU.mult)
    def ring(n, shape, dt, name):
        return [P.tile(shape, dt, f"{name}{i}", es=es_b) for i in range(n)]

    ug = ring(3, [128, 512], F32, "ug"); vg = ring(2, [128, 512], F32, "vg")
    vbf = ring(2, [128, 512], BF16, "vbf"); oa = ring(2, [128, 512], F32, "oa")
    oan = ring(2, [128, 512], BF16, "oan"); mTa = ring(2, [128, 4, 128], BF16, "mTa")
    xsb = ring(2, [128, 1024], F32, "xsb"); xsb2 = ring(2, [128, 1024], F32, "xsc")
    xnbt = ring(2, [128, 1024], BF16, "xnbt"); xnTt = ring(2, [128, 8, 128], BF16, "xnTt")
    sqb = P.tile([128, 1024], BF16, "sqb", es=es_b)
    NS = 3
    ssb = [ring(NS, [128, 1], F32, f"ssb{j}_") for j in range(3)]
    stb = [ring(NS, [128, 1], F32, f"stb{j}_") for j in range(3)]
    srb = [ring(NS, [128, 1], F32, f"srb{j}_") for j in range(3)]
    es_ps = ExitStack()
    psU = [P.ptile([128, 512], F32, f"psU{i}", es=es_ps) for i in range(2)]
    psV = P.ptile([128, 512], F32, "psV", es=es_ps)
    psZ = P.ptile([128, 512], F32, "psZ", es=es_ps)
    psX = P.ptile([128, 8, 128], BF16, "psX", es=es_ps)
    psD = P.ptile([128, 1024], F32, "psD", es=es_ps)
    psTt = P.ptile([128, 8, 128], BF16, "psTt", es=es_ps)

    def rstd3(j, t, n):
        rstd_from_ss(ssb[j][t % NS], n, stb[j][t % NS], srb[j][t % NS])

    def b_s0(t):
        P.dma(xsb[t % 2], I["x"][t * 128:(t + 1) * 128, :])
        P.actf(sqb, xsb[t % 2], AF.Square, accum=ssb[0][t % NS])
        rstd3(0, t, 1024)

    def b_s1(t):
        P.stt(xnbt[t % 2], xsb[t % 2], srb[0][t % NS], gbc, ALU.mult, ALU.mult)
        for dc in range(8):
            P.tr(psTt[:, dc, :], xnbt[t % 2][:, dc * 128:(dc + 1) * 128], identb)

    def b_s1b(t):
        P.cp(xnTt[t % 2], psTt, eng="dve")

    def b_s2(t):
        for dc in range(8):
            P.mm(psU[t % 2], xnTt[t % 2][:, dc, :], Winu[:, dc, :], dc == 0, dc == 7)
        for dc in range(8):
            P.mm(psV, xnTt[t % 2][:, dc, :], Winv[:, dc, :], dc == 0, dc == 7)

    def b_s3(t):
        P.actf(vg[t % 2], psV, GELU)
        P.actf(ug[t % 3], psU[t % 2], GELU)
        P.actf(sqb[:, 0:512], vg[t % 2], AF.Square, accum=ssb[1][t % NS])
        rstd3(1, t, 512)

    def b_s4(t):
        P.stt(vbf[t % 2], vg[t % 2], srb[1][t % NS], sgu_bc, ALU.mult, ALU.mult)
        for hh in range(8):
            P.mm(psZ[:, hh * 64:(hh + 1) * 64], wsTb[:, hh, :], vbf[t % 2][:, hh * 64:(hh + 1) * 64], True, True)

    def b_s5(t):
        P.tt(oa[t % 2], psZ, bsp, ALU.add)
        P.tt(oa[t % 2], oa[t % 2], ug[t % 3], ALU.mult)
        P.actf(sqb[:, 0:512], oa[t % 2], AF.Square, accum=ssb[2][t % NS])
        rstd3(2, t, 512)

    def b_s6(t):
        P.stt(oan[t % 2], oa[t % 2], srb[2][t % NS], goa_bc, ALU.mult, ALU.mult)
        for ct in range(4):
            P.tr(psX[:, ct, :], oan[t % 2][:, ct * 128:(ct + 1) * 128], identb)

    def b_s6b(t):
        P.cp(mTa[t % 2], psX[:, 0:4, :], eng="act")
        P.dma(xsb2[t % 2], I["x"][t * 128:(t + 1) * 128, :])

    def b_s7(t):
        cols = slice(t * 128, (t + 1) * 128)
        for hh in range(2):
            o = psD[:, hh * 512:(hh + 1) * 512]
            for kk in range(8):
                lhs = mTa[t % 2][:, kk, :] if kk < 4 else mixTb[:, kk - 4, cols]
                P.mm(o, lhs, Wout[:, kk, hh * 512:(hh + 1) * 512], kk == 0, kk == 7)
        P.tt(h_tm[:, t, :].rk(("h", t)), xsb2[t % 2], psD, ALU.add)

    pipeline(16, [b_s0, b_s1, b_s1b, b_s2, b_s3, b_s4, b_s5, b_s6, b_s6b, b_s7])
    P.barrier()
    es_ps.close()
    es_b.close()
    es_p1.close()
    if "h1" in D:
        P.dma(D["h1"].rearrange("(t p) d -> p t d", p=128), h_tm)
        P.barrier()
    if STOP_AFTER == "B":
        P.emit(final_wait_keys=["OUT"])
        return nc

    xnT = P.tile([128, 8, NTOK], BF16, "xnT")
    Wpg = P.tile([128, 8, 1024], BF16, "Wpg")
    Wpp = P.tile([128, 2, 1024], BF16, "Wpp")
    es_m = ExitStack()
    load_gain(1)
    br_bc = P.tile([128, 36], F32, "br_bc", es=es_m)
    P.dma(br_bc, I["br"].partition_broadcast(128).rearrange("p a n -> p (a n)"))
    logit = P.tile([128, 16, 36], F32, "logit", es=es_m)
    comb = P.tile([128, 16, 4, 8], F32, "comb", es=es_m)
    NB = 3
    Wg = [P.tile([128, 8, 256], BF16, f"Wg{i}", es=es_m) for i in range(NB)]
    Wu = [P.tile([128, 8, 256], BF16, f"Wu{i}", es=es_m) for i in range(NB)]
    Wd = [P.tile([128, 2, 1024], BF16, f"Wd{i}", es=es_m) for i in range(NB)]

    def load_expert(e):
        b = e % NB
        P.dma(Wg[b], I["wg"][e].rearrange("(c p) f -> p c f", p=128), q="pool")
        P.dma(Wu[b], I["wu"][e].rearrange("(c p) f -> p c f", p=128), q="pool")
        P.dma(Wd[b], I["wd"][e].rearrange("(c p) f -> p c f", p=128), q="pool")

    for e in range(min(NB - 1, N_EXPERTS_RUN)):
        load_expert(e)
    es_n = ExitStack()
    wrf = P.tile([128, 8, 36], F32, "wrf", es=es_n)
    P.dma(wrf, I["wr"].rearrange("(c p) f -> p c f", p=128))
    wrh = P.tile([128, 8, 36], BF16, "wrh", es=es_n); wrl = P.tile([128, 8, 36], BF16, "wrl", es=es_n)
    P.cp(wrh, wrf); P.tt(wrl, wrf, wrh, ALU.subtract)

    def ringn(n, shape, dt, name):
        return [P.tile(shape, dt, f"{name}{i}", es=es_n) for i in range(n)]

    xfs = ringn(3, [128, 1024], F32, "xf")
    sq2 = P.tile([128, 1024], BF16, "sq2", es=es_n)
    xhi = ringn(2, [128, 1024], BF16, "xhi"); xlo = ringn(2, [128, 1024], BF16, "xlo")
    loTt = ringn(2, [128, 8, 128], BF16, "loTt")
    ss2 = ringn(3, [128, 1], F32, "ss2_"); st2 = ringn(3, [128, 1], F32, "st2_"); sr2 = ringn(3, [128, 1], F32, "sr2_")
    es_ps = ExitStack()
    psH = [P.ptile([128, 8, 128], BF16, f"psH{i}", es=es_ps) for i in range(2)]
    psLo = [P.ptile([128, 8, 128], BF16, f"psLo{i}", es=es_ps) for i in range(2)]
    psLG = [P.ptile([128, 512], F32, f"psLG{i}", es=es_ps) for i in range(2)]

    def hk2(t):
        return h_tm[:, t, :].rk(("h", t))

    def xk2(t):
        return xnT[:, :, t * 128:(t + 1) * 128].rk(("xnT2", t // 4))

    def n_s0(t):
        P.actf(sq2, hk2(t), AF.Square, accum=ss2[t % 3])
        rstd_from_ss(ss2[t % 3], 1024, st2[t % 3], sr2[t % 3])

    def n_s1(t):
        P.stt(xfs[t % 3], hk2(t), sr2[t % 3], gbc, ALU.mult, ALU.mult)
        P.cp(xhi[t % 2], xfs[t % 3], eng="act")
        P.tt(xlo[t % 2], xfs[t % 3], xhi[t % 2], ALU.subtract)

    def n_s2(t):
        for dc in range(8):
            P.tr(psH[t % 2][:, dc, :], xhi[t % 2][:, dc * 128:(dc + 1) * 128], identb)
        for dc in range(8):
            P.tr(psLo[t % 2][:, dc, :], xlo[t % 2][:, dc * 128:(dc + 1) * 128], identb)

    def n_s2b(t):
        P.add("act", lambda e, t=t: e.copy(out=xk2(t).ap, in_=psH[t % 2].ap), reads=list(psH[t % 2].keys), pwrites=list(xk2(t).keys))
        P.cp(loTt[t % 2], psLo[t % 2], eng="dve")

    def n_s3(t):
        terms = []
        for dc in range(8):
            terms += [(xk2(t)[:, dc, :], wrh[:, dc, :]), (xk2(t)[:, dc, :], wrl[:, dc, :]), (loTt[t % 2][:, dc, :], wrh[:, dc, :])]
        for n_, (l_, r_) in enumerate(terms):
            P.mm(psLG[t % 2][:, 0:36], l_, r_, n_ == 0, n_ == len(terms) - 1)
        P.tt(logit[:, t, :], psLG[t % 2][:, 0:36], br_bc, ALU.add, pw=True)

    pipeline(16, [n_s0, n_s1, n_s2, n_s2b, n_s3])
    P.barrier()
    es_ps.close()
    es_n.close()
    rt = lambda shape, key=None: P.tile(shape, F32, key, es=es_m)
    AXX = mybir.AxisListType.X

    def reduce_x(out, in_, op):
        P.add("dve", lambda e: e.tensor_reduce(out=out.ap, in_=in_.ap, axis=AXX, op=op),
              reads=list(in_.keys), writes=list(out.keys))

    lc = logit[:, :, 0:4]
    mxc = rt([128, 16]); reduce_x(mxc, lc, ALU.max)
    ec = rt([128, 16, 4]); P.tt(ec, lc, mxc.us(2).bc((128, 16, 4)), ALU.subtract)
    ohg = rt([128, 16, 4]); P.ts(ohg, ec, 1e30, ALU.mult, s2=1.0, op1=ALU.add)
    P.ts(ohg, ohg, 0.0, ALU.max, s2=1.0, op1=ALU.min)
    P.actf(ec, ec, AF.Exp)
    sec = rt([128, 16]); reduce_x(sec, ec, ALU.add)
    pg = rt([128, 16]); P.recip(pg, sec)
    lfa = logit[:, :, 4:36].re("p t (g e) -> p t g e", g=4)
    msk = rt([128, 16, 4, 8]); P.tt(msk, lfa, ohg.us(3).bc((128, 16, 4, 8)), ALU.mult)
    lf = rt([128, 16, 8])
    P.tt(lf, msk[:, :, 0, :], msk[:, :, 1, :], ALU.add)
    P.tt(lf, lf, msk[:, :, 2, :], ALU.add); P.tt(lf, lf, msk[:, :, 3, :], ALU.add)
    top8 = rt([128, 16, 8])
    for t in range(16):
        P.add("dve", lambda e, t=t: e.max(out=top8.ap[:, t, :], in_=lf.ap[:, t, :]),
              reads=list(lf.keys), pwrites=list(top8.keys))
    m1 = top8[:, :, 0:1]; m2_ = top8[:, :, 1:2]
    ef = rt([128, 16, 8]); P.tt(ef, lf, m1.bc((128, 16, 8)), ALU.subtract)
    P.actf(ef, ef, AF.Exp)
    mk2 = rt([128, 16, 8]); P.tt(mk2, lf, m2_.bc((128, 16, 8)), ALU.subtract)
    P.ts(mk2, mk2, 1e30, ALU.mult, s2=1.0, op1=ALU.add)
    P.ts(mk2, mk2, 0.0, ALU.max, s2=1.0, op1=ALU.min)
    P.tt(ef, ef, mk2, ALU.mult)
    den2 = rt([128, 16]); reduce_x(den2, ef, ALU.add)
    rd2 = rt([128, 16]); P.recip(rd2, den2)
    P.tt(rd2, rd2, pg, ALU.mult)
    P.tt(ef, ef, rd2.us(2).bc((128, 16, 8)), ALU.mult)
    P.tt(comb, ohg.us(3).bc((128, 16, 4, 8)), ef.us(2).bc((128, 16, 4, 8)), ALU.mult)
    if "comb" in D:
        P.dma(D["comb"].rearrange("(t p) e -> p t e", p=128), comb.re("p t g e -> p t (g e)"))
    he = [P.tile([128, 2, 512], BF16, f"he{i}", es=es_m) for i in range(2)]
    gsb = [P.tile([128, 512], BF16, f"gsb{i}", es=es_m) for i in range(2)]
    es_ps = ExitStack()
    psG = [P.ptile([128, 512], F32, f"psG{i}", es=es_ps) for i in range(2)]
    psUu = [P.ptile([128, 512], F32, f"psUu{i}", es=es_ps) for i in range(2)]
    psDn = [P.ptile([128, 1024], F32, f"psDn{i}", es=es_ps) for i in range(2)]
    dn_cnt = [0]

    def down_tile(e, tb, tl):
        b = e % NB
        hb = he[tb % 2]
        tt_i = tb * 4 + tl
        pd = psDn[dn_cnt[0] % 2]; dn_cnt[0] += 1
        for hh in range(2):
            for ft in range(2):
                P.mm(pd[:, hh * 512:(hh + 1) * 512], hb[:, ft, tl * 128:(tl + 1) * 128],
                     Wd[b][:, ft, hh * 512:(hh + 1) * 512], ft == 0, ft == 1)
        hk = h_tm[:, tt_i, :].rk(("h", tt_i))
        P.stt(hk, pd, comb[:, tt_i, e // 8, (e % 8):(e % 8) + 1], hk, ALU.mult, ALU.add)

    for e in range(N_EXPERTS_RUN):
        b = e % NB
        for tb in range(4):
            if tb == 1 and e + NB - 1 < N_EXPERTS_RUN:
                load_expert(e + NB - 1)
            if tb == 2 and e == min(4, N_EXPERTS_RUN - 1):
                P.dma(Wpg, I["w_pg"].rearrange("(c p) f -> p c f", p=128), q="pool")
                P.dma(Wpp, I["w_pp"].rearrange("(c p) f -> p c f", p=128), q="pool")
            xk = xnT[:, :, tb * 512:(tb + 1) * 512].rk(("xnT2", tb))
            if tb >= 1:
                pend = [(e, tb - 1, tl) for tl in range(4)]
            elif e >= 1:
                pend = [(e - 1, 3, tl) for tl in range(4)]
            else:
                pend = []
            for ft in range(2):
                pg_ = psG[ft]; pu_ = psUu[ft]
                for dc in range(8):
                    P.mm(pg_, Wg[b][:, dc, ft * 128:(ft + 1) * 128], xk[:, dc, :], dc == 0, dc == 7)
                if pend:
                    down_tile(*pend.pop(0))
                for dc in range(8):
                    P.mm(pu_, Wu[b][:, dc, ft * 128:(ft + 1) * 128], xk[:, dc, :], dc == 0, dc == 7)
                P.actf(gsb[ft], pg_, AF.Silu)
                P.tt(he[tb % 2][:, ft, :], gsb[ft], pu_, ALU.mult, pw=True)
                if pend:
                    down_tile(*pend.pop(0))
    for tl in range(4):
        down_tile(N_EXPERTS_RUN - 1, 3, tl)
    P.barrier()
    es_ps.close()
    es_m.close()
    if "h2" in D:
        P.dma(D["h2"].rearrange("(t p) d -> p t d", p=128), h_tm)
        P.barrier()
    if STOP_AFTER == "moe":
        P.emit(final_wait_keys=["OUT"])
        return nc

    es_3 = ExitStack()
    load_gain(2)
    gbc2 = P.tile([128, 1024], F32, "gbc2", es=es_3)
    P.dma(gbc2, I["gvec"][3:4, :].partition_broadcast(128).rearrange("p a n -> p (a n)"))

    def ring3(n, shape, dt, name):
        return [P.tile(shape, dt, f"{name}{i}", es=es_3) for i in range(n)]

    pst = ring3(2, [128, 256], F32, "ptst"); pbf = ring3(2, [128, 256], BF16, "pbf")
    xh3 = ring3(2, [128, 1024], BF16, "xh3"); x3T = ring3(2, [128, 8, 128], BF16, "x3T")
    pTt = ring3(2, [128, 2, 128], BF16, "pTt")
    gts = ring3(2, [128, 1024], F32, "gts"); ost = ring3(2, [128, 1024], F32, "ost")
    sq3 = P.tile([128, 1024], BF16, "sq3", es=es_3)
    NS3 = 3
    ss3 = [ring3(NS3, [128, 1], F32, f"ss3{j}_") for j in range(2)]
    st3 = [ring3(NS3, [128, 1], F32, f"st3{j}_") for j in range(2)]
    sr3 = [ring3(NS3, [128, 1], F32, f"sr3{j}_") for j in range(2)]
    es_ps = ExitStack()
    psT3 = P.ptile([128, 8, 128], BF16, "psT3", es=es_ps)
    psP3 = P.ptile([128, 8, 128], BF16, "psP3", es=es_ps)
    psGt = [P.ptile([128, 1024], F32, f"psGt{i}", es=es_ps) for i in range(2)]
    psPp = P.ptile([128, 1024], F32, "psPp", es=es_ps)

    def hk_(t):
        return h_tm[:, t, :].rk(("h", t))

    def c_s0(t):
        P.dma(pst[t % 2], I["p"][t * 128:(t + 1) * 128, :])
        P.actf(sq3, hk_(t), AF.Square, accum=ss3[0][t % NS3])
        rstd_from_ss(ss3[0][t % NS3], 1024, st3[0][t % NS3], sr3[0][t % NS3])

    def c_s1(t):
        P.stt(xh3[t % 2], hk_(t), sr3[0][t % NS3], gbc, ALU.mult, ALU.mult)
        P.cp(pbf[t % 2], pst[t % 2], eng="act")
        for dc in range(8):
            P.tr(psT3[:, dc, :], xh3[t % 2][:, dc * 128:(dc + 1) * 128], identb)
        for kk in range(2):
            P.tr(psP3[:, kk, :], pbf[t % 2][:, kk * 128:(kk + 1) * 128], identb)

    def c_s1b(t):
        P.cp(x3T[t % 2], psT3, eng="dve")
        P.cp(pTt[t % 2], psP3[:, 0:2, :], eng="act")

    def c_s2(t):
        for hh in range(2):
            for dc in range(8):
                P.mm(psGt[t % 2][:, hh * 512:(hh + 1) * 512], x3T[t % 2][:, dc, :], Wpg[:, dc, hh * 512:(hh + 1) * 512], dc == 0, dc == 7)
        for hh in range(2):
            for kk in range(2):
                P.mm(psPp[:, hh * 512:(hh + 1) * 512], pTt[t % 2][:, kk, :], Wpp[:, kk, hh * 512:(hh + 1) * 512], kk == 0, kk == 1)

    def c_s3(t):
        P.actf(gts[t % 2], psGt[t % 2], AF.Sigmoid)
        P.tt(gts[t % 2], gts[t % 2], psPp, ALU.mult)
        P.tt(hk_(t), hk_(t), gts[t % 2], ALU.add)

    def c_s4(t):
        P.actf(sq3, hk_(t), AF.Square, accum=ss3[1][t % NS3])
        rstd_from_ss(ss3[1][t % NS3], 1024, st3[1][t % NS3], sr3[1][t % NS3])

    def c_s5(t):
        P.stt(ost[t % 2], hk_(t), sr3[1][t % NS3], gbc2, ALU.mult, ALU.mult)
        P.dma(out_d[t * 128:(t + 1) * 128, :], ost[t % 2])

    pipeline(16, [c_s0, c_s1, c_s1b, c_s2, c_s3, c_s4, c_s5])
    P.barrier()
    es_ps.close()
    es_3.close()
    P.emit(final_wait_keys=["OUT"])
    return nc


def _host_consts():
    c = {}
    c["ident"] = np.eye(128, dtype=np.float32)
    s = np.arange(128)
    c["maskS"] = (s[:, None] <= s[None, :]).astype(np.float32)
    i_of = s // 16
    c["maskM"] = (i_of[None, :] >= i_of[:, None]).astype(np.float32)
    hm = np.zeros((128, 2), np.float32); hm[:64, 0] = 1; hm[64:, 1] = 1
    c["hmask"] = hm
    return c


def _pg(a):
    return np.ascontiguousarray(a.reshape(16, 2, 64).transpose(1, 2, 0).reshape(128, 16))


def _pgc(a):
    return np.ascontiguousarray(a.reshape(16, 2, 64, 16).transpose(1, 2, 0, 3).reshape(128, 256))


def make_in_maps(inputs):
    f = lambda k: np.asarray(inputs[k], dtype=np.float32)
    x = f("x"); p = f("p")[0]
    shared = _host_consts()
    shared["w_in"] = np.ascontiguousarray(f("w_in")[0]); shared["w_glu"] = np.ascontiguousarray(f("w_glu")[0])
    shared["w_out"] = np.ascontiguousarray(f("w_out")[0]); shared["w_pg"] = np.ascontiguousarray(f("w_ple_gate")[0])
    shared["w_pp"] = np.ascontiguousarray(f("w_ple_proj")[0])
    shared["wg"] = np.ascontiguousarray(f("w_gate_e")[0].reshape(32, 1024, 256))
    shared["wu"] = np.ascontiguousarray(f("w_up_e")[0].reshape(32, 1024, 256))
    shared["wd"] = np.ascontiguousarray(f("w_down_e")[0].reshape(32, 256, 1024))
    shared["wr"] = np.ascontiguousarray(np.concatenate([f("w_coarse")[0]] + [f("w_fine")[0, g] for g in range(4)], axis=1))
    shared["br"] = np.ascontiguousarray(np.concatenate([f("b_coarse")[0], f("b_fine")[0].reshape(-1)])[None, :])
    gv = np.zeros((6, 1024), np.float32)
    gv[0] = f("norm1")[0]; gv[1] = f("norm2")[0]; gv[2] = f("norm3")[0]; gv[3] = f("final_norm")
    gv[4, :512] = f("out_norm_a")[0]; gv[4, 512:] = f("out_norm_b")[0]
    gv[5, :512] = f("sgu_norm")[0]; gv[5, 512:] = f("d_skip")[0]
    shared["gvec"] = gv
    shared["gT"] = np.ascontiguousarray(f("norm1")[0].reshape(8, 128).T)
    bs = f("b_spatial")[0]
    shared["bsp"] = np.ascontiguousarray(np.repeat(bs.T[:, :, None], 64, axis=2).reshape(128, 512))
    shared["wsT"] = np.ascontiguousarray(f("w_spatial")[0].transpose(0, 2, 1))
    shared["are"] = _pg(f("a_re")[0]); shared["aim"] = _pg(f("a_im")[0])
    shared["ldt"] = _pg(np.repeat(f("log_dt")[0][:, None], 64, axis=1))
    shared["bre"] = _pgc(f("b_re")[0]); shared["bim"] = _pgc(f("b_im")[0])
    shared["cre"] = _pgc(f("c_re")[0].transpose(0, 2, 1)); shared["cim"] = _pgc(f("c_im")[0].transpose(0, 2, 1))
    shared["bglu"] = np.ascontiguousarray(f("b_glu"))
    in_maps = []
    for c in range(8):
        b, half = c // 2, c % 2
        m = dict(shared)
        m["x"] = np.ascontiguousarray(x[b, half * NTOK:(half + 1) * NTOK])
        m["xprev"] = np.ascontiguousarray(x[b, 0:NTOK]) if half == 1 else np.zeros((NTOK, 1024), np.float32)
        m["p"] = np.ascontiguousarray(p[b, half * NTOK:(half + 1) * NTOK])
        in_maps.append(m)
    return in_maps


def run(inputs, dbg_specs=None, cores=8):
    nc = bass.Bass("TRN2", target_bir_lowering=False)
    build_program(nc, dbg_specs or {})
    in_maps = make_in_maps(inputs)[:cores]
    res = run_bass_kernel_spmd(nc, in_maps, core_ids=list(range(cores)))
    return res.results


def kernel(**inputs):
    results = run(inputs)
    out = np.zeros((4, 4096, 1024), np.float32)
    for c in range(8):
        b, half = c // 2, c % 2
        out[b, half * NTOK:(half + 1) * NTOK] = results[c]["out"]
    return out
```
